# Optimizing a Trainium2 kernel written in Bass

```python
import jax
import jax.numpy as jnp
from jax import lax
import numpy as np

D_MODEL = 1024
BATCH = 32
SEQ = 2048
DEPTH = 1

CHUNK = 64
Q_BLOCK = 128
PLE_DIM = 256
EPS = 1e-6
ROPE_THETA = 500000.0

RET_HEADS = 8
RET_DK = 64
RET_DV = 64
RET_THETA = 10000.0
RET_W = RET_HEADS * RET_DV

DSA_HEADS = 8
DSA_HEAD_DIM = 64
DSA_ROPE = DSA_HEAD_DIM // 4
DSA_NOPE = DSA_HEAD_DIM - DSA_ROPE
DSA_LATENT = 128
DSA_VDIM = 64
DSA_W = DSA_HEADS * DSA_VDIM
TOPK_MAX = 256

IDX_HEADS = 8
IDX_DIM = 32
IDX_ROPE = IDX_DIM // 4

MIX_W = RET_W + DSA_W

IN_SPLITS = (RET_HEADS * RET_DK, RET_HEADS * RET_DK, RET_W, RET_W,
             DSA_HEADS * DSA_HEAD_DIM, DSA_LATENT, DSA_ROPE, DSA_W,
             IDX_HEADS * IDX_DIM, IDX_DIM, IDX_HEADS)
IN_WIDTH = sum(IN_SPLITS)

kernel_name = "hybrid_retention_dsa_block"


def rms_norm(x, g):
    xf = x.astype(jnp.float32)
    y = xf * lax.rsqrt(jnp.mean(xf * xf, -1, keepdims=True) + EPS)
    return (y * g.astype(jnp.float32)).astype(x.dtype)


def head_group_norm(o, g):
    of = o.astype(jnp.float32)
    mu = jnp.mean(of, -1, keepdims=True)
    var = jnp.mean(jnp.square(of - mu), -1, keepdims=True)
    y = ((of - mu) * lax.rsqrt(var + EPS)).reshape(o.shape[:-2] + (-1,))
    return (y * g.astype(jnp.float32)).astype(o.dtype)


def rotary(x, pos, theta):
    r = x.shape[-1]
    inv = theta ** (-jnp.arange(0, r, 2, dtype=jnp.float32) / r)
    ang = pos.astype(jnp.float32)[:, None] * inv[None, :]
    shape = (ang.shape[0],) + (1,) * (x.ndim - 3) + (r // 2,)
    cos = jnp.cos(ang).reshape(shape)
    sin = jnp.sin(ang).reshape(shape)
    xf = x.astype(jnp.float32)
    x1, x2 = xf[..., : r // 2], xf[..., r // 2:]
    out = jnp.concatenate([x1 * cos - x2 * sin, x2 * cos + x1 * sin], -1)
    return out.astype(x.dtype)


def partial_rotary(x, pos, n_rot, theta):
    return jnp.concatenate([rotary(x[..., :n_rot], pos, theta), x[..., n_rot:]], -1)


def retention(q, k, v):
    B, S, H, dk = q.shape
    dv = v.shape[-1]
    nC = S // CHUNK
    dt = q.dtype
    log_g = jnp.log1p(-jnp.exp2(-5.0 - jnp.arange(H, dtype=jnp.float32)))
    n = jnp.arange(CHUNK, dtype=jnp.float32)
    intra = jnp.exp(log_g[:, None, None] * jnp.abs(n[:, None] - n[None, :]))
    zeta = jnp.exp(log_g[:, None] * (CHUNK - 1 - n)[None, :])
    xi = jnp.exp(log_g[:, None] * (n + 1.0)[None, :])
    chunk_decay = jnp.exp(log_g * CHUNK)[:, None, None]

    qc = q.reshape(B, nC, CHUNK, H, dk) * (dk ** -0.5)
    kc = k.reshape(B, nC, CHUNK, H, dk)
    vc = v.reshape(B, nC, CHUNK, H, dv)

    scores = jnp.einsum('bcnhd,bcmhd->bchnm', qc, kc) * intra.astype(dt)
    o_intra = jnp.einsum('bchnm,bcmhe->bcnhe', scores, vc)

    kv = jnp.einsum('bcmhd,bcmhe,hm->cbhde', kc, vc, zeta.astype(dt)).astype(jnp.float32)

    def step(state, kv_c):
        return chunk_decay * state + kv_c, state

    _, prev = lax.scan(step, jnp.zeros((B, H, dk, dv), jnp.float32), kv)
    o_cross = jnp.einsum('bcnhd,cbhde,hn->bcnhe', qc, prev.astype(dt), xi.astype(dt))
    return (o_intra + o_cross).reshape(B, S, H, dv)


def sparse_attention(q_nope, q_pe, c_kv, k_pe, q_idx, k_idx, w_idx, w_uk, w_uv):
    B, S = c_kv.shape[:2]
    n_sel = min(TOPK_MAX, S // 4)
    n_blk = S // Q_BLOCK
    q_lat = jnp.einsum('bshn,hnc->bshc', q_nope, w_uk)
    key_chunk = jnp.arange(S) // CHUNK
    scale = DSA_HEAD_DIM ** -0.5
    idx_scale = IDX_DIM ** -0.5
    gather = jax.vmap(lambda table, ids: table[ids])

    def to_blocks(a):
        return jnp.moveaxis(a.reshape((B, n_blk, Q_BLOCK) + a.shape[2:]), 1, 0)

    def block(args):
        blk, ql, qp, qi, wi = args
        t = blk * Q_BLOCK + jnp.arange(Q_BLOCK)
        valid = key_chunk[None, :] <= (t // CHUNK)[:, None]
        rel = jax.nn.relu(jnp.einsum('bthd,bsd->bths', qi, k_idx) * idx_scale)
        score = jnp.einsum('bth,bths->bts', wi, rel).astype(jnp.float32)
        score = jnp.where(valid[None], score, -jnp.inf)
        top_val, top_idx = lax.top_k(score, n_sel)
        sel_ok = jnp.isfinite(top_val)
        c_sel = gather(c_kv, top_idx)
        pe_sel = gather(k_pe, top_idx)
        logits = (jnp.einsum('bthc,btkc->bthk', ql, c_sel)
                  + jnp.einsum('bthr,btkr->bthk', qp, pe_sel)).astype(jnp.float32) * scale
        logits = jnp.where(sel_ok[:, :, None, :], logits, -jnp.inf)
        prob = jax.nn.softmax(logits, axis=-1).astype(c_kv.dtype)
        o_lat = jnp.einsum('bthk,btkc->bthc', prob, c_sel)
        return jnp.einsum('bthc,hcd->bthd', o_lat, w_uv)

    out = lax.map(block, (jnp.arange(n_blk), to_blocks(q_lat), to_blocks(q_pe),
                          to_blocks(q_idx), to_blocks(w_idx)))
    return jnp.moveaxis(out, 0, 1).reshape(B, S, DSA_HEADS, DSA_VDIM)


def setup_inputs(seed: int = 0) -> dict:
    key = jax.random.key(seed)
    ks = jax.random.split(key, 13)
    f32 = jnp.float32

    def normal(k, shape, scale):
        return jax.random.normal(k, shape, f32) * scale

    def gain(k, shape):
        return 1.0 + 0.01 * jax.random.normal(k, shape, f32)

    return {
        "x": normal(ks[0], (BATCH, SEQ, D_MODEL), 1.0),
        "p": normal(ks[1], (DEPTH, BATCH, SEQ, PLE_DIM), 1.0),
        "positions": jnp.arange(SEQ, dtype=jnp.int32),
        "norm_mix": gain(ks[2], (DEPTH, D_MODEL)),
        "w_in": normal(ks[3], (DEPTH, D_MODEL, IN_WIDTH), D_MODEL ** -0.5),
        "ret_norm": gain(ks[4], (DEPTH, RET_W)),
        "kv_norm": gain(ks[5], (DEPTH, DSA_LATENT)),
        "w_uk": normal(ks[6], (DEPTH, DSA_HEADS, DSA_NOPE, DSA_LATENT), DSA_NOPE ** -0.5),
        "w_uv": normal(ks[7], (DEPTH, DSA_HEADS, DSA_LATENT, DSA_VDIM), DSA_LATENT ** -0.5),
        "w_out": normal(ks[8], (DEPTH, MIX_W, D_MODEL), MIX_W ** -0.5),
        "norm_ple": gain(ks[9], (DEPTH, D_MODEL)),
        "w_ple_gate": normal(ks[10], (DEPTH, D_MODEL, D_MODEL), D_MODEL ** -0.5),
        "w_ple_proj": normal(ks[11], (DEPTH, PLE_DIM, D_MODEL), PLE_DIM ** -0.5),
        "norm_final": gain(ks[12], (D_MODEL,)),
    }


def reference(x, p, positions, norm_mix, w_in, ret_norm, kv_norm, w_uk, w_uv, w_out,
              norm_ple, w_ple_gate, w_ple_proj, norm_final):
    B, S, _ = x.shape
    split_at = np.cumsum(IN_SPLITS)[:-1].tolist()
    for i in range(DEPTH):
        h = rms_norm(x, norm_mix[i])
        proj = h @ w_in[i]
        (r_q, r_k, r_v, r_g, d_q, d_c, d_pe, d_g,
         i_q, i_k, i_w) = jnp.split(proj, split_at, axis=-1)

        r_q = rotary(r_q.reshape(B, S, RET_HEADS, RET_DK), positions, RET_THETA)
        r_k = rotary(r_k.reshape(B, S, RET_HEADS, RET_DK), positions, RET_THETA)
        r_v = r_v.reshape(B, S, RET_HEADS, RET_DV)
        ret = head_group_norm(retention(r_q, r_k, r_v), ret_norm[i]) * jax.nn.silu(r_g)

        d_q = d_q.reshape(B, S, DSA_HEADS, DSA_HEAD_DIM)
        q_pe = rotary(d_q[..., :DSA_ROPE], positions, ROPE_THETA)
        q_nope = d_q[..., DSA_ROPE:]
        c_kv = rms_norm(d_c, kv_norm[i])
        k_pe = rotary(d_pe, positions, ROPE_THETA)
        q_idx = partial_rotary(i_q.reshape(B, S, IDX_HEADS, IDX_DIM), positions, IDX_ROPE, ROPE_THETA)
        k_idx = partial_rotary(i_k, positions, IDX_ROPE, ROPE_THETA)
        w_idx = i_w * (IDX_HEADS ** -0.5)
        dsa = sparse_attention(q_nope, q_pe, c_kv, k_pe, q_idx, k_idx, w_idx,
                               w_uk[i], w_uv[i]).reshape(B, S, DSA_W) * jax.nn.silu(d_g)

        x = x + jnp.concatenate([ret, dsa], axis=-1) @ w_out[i]

        gate = jax.nn.sigmoid(rms_norm(x, norm_ple[i]) @ w_ple_gate[i])
        x = x + gate * (p[i] @ w_ple_proj[i])
    return rms_norm(x, norm_final)
```

```python
import math
from contextlib import ExitStack

import numpy as np
import concourse.bass as bass
import concourse.mybir as mybir
from concourse.bass_utils import run_bass_kernel_spmd

F32 = mybir.dt.float32
BF16 = mybir.dt.bfloat16
I32 = mybir.dt.int32
AF = mybir.ActivationFunctionType
ALU = mybir.AluOpType
AX = mybir.AxisListType

N_CORES = 8
BATCH = 32
SEQ = 2048
DM = 1024
NT = SEQ // 128
PLE = 256
EPS = 1e-6
IN_W = 3512
N_BIS = 22
NEG = -30000.0
MAGIC = 12582912.0
TWO_PI = 2.0 * math.pi


class Buf:
    def __init__(self, name):
        self.name = name
        self.w = None
        self.r = {}


class TL:
    def __init__(self, t, shape, name):
        self.t = t
        self.shape = list(shape)
        self.b = Buf(name)
        self.row = int(np.prod(shape[1:]))

    def ap(self, p0, pn, off, dims):
        return bass.AP(self.t, p0 * self.row + off, [[self.row, pn]] + [list(d) for d in dims])

    def __getitem__(self, k):
        return self.t[k]


class Sched:
    ENG = ('pe', 'dve', 'act', 'pool', 'sp')

    def __init__(self, nc, es):
        self.nc = nc
        self.es = es
        self.q = {e: [] for e in self.ENG}
        self.sem = {e: es.enter_context(nc.semaphore("s_" + e)) for e in self.ENG if e != 'sp'}
        self.cnt = {e: 0 for e in self.sem}
        self.seen = {e: {} for e in self.ENG}
        self.dsem = {}
        self.dcnt = {}

    def _semobj(self, key):
        return self.sem[key] if key in self.sem else self.dsem[key]

    def _deps(self, e, reads, writes):
        need = {}

        def add(tok, kind):
            if tok is None:
                return
            k, v = tok
            if k == e and (e == 'pe' or kind != 'raw'):
                return
            if need.get(k, 0) < v:
                need[k] = v
        for b in reads:
            add(b.w, 'raw')
        for b in writes:
            add(b.w, 'waw')
            for k, v in b.r.items():
                add((k, v), 'war')
        out = []
        for k, v in need.items():
            if self.seen[e].get(k, 0) < v:
                self.seen[e][k] = v
                out.append((k, v))
        return out

    def _mark(self, tok, key, reads, writes):
        for b in reads:
            if b.r.get(key, 0) < tok[1]:
                b.r[key] = tok[1]
        for b in writes:
            b.w = tok
            b.r = {}

    def op(self, e, fn, reads=(), writes=()):
        reads = [t.b for t in reads]
        writes = [t.b for t in writes]
        waits = self._deps(e, reads, writes)
        self.cnt[e] += 1
        tok = (e, self.cnt[e])
        self.q[e].append((waits, fn, (e, 1)))
        self._mark(tok, e, reads, writes)
        return tok

    def dma(self, e, fn, semkey, reads=(), writes=()):
        reads = [t.b for t in reads]
        writes = [t.b for t in writes]
        if semkey not in self.dsem:
            self.dsem[semkey] = self.es.enter_context(self.nc.semaphore("d_" + semkey))
            self.dcnt[semkey] = 0
        waits = self._deps(e, reads, writes)
        self.dcnt[semkey] += 16
        tok = (semkey, self.dcnt[semkey])
        self.q[e].append((waits, fn, (semkey, 16)))
        self._mark(tok, semkey, reads, writes)
        return tok

    def wait_tok(self, e, tok):
        if self.seen[e].get(tok[0], 0) < tok[1]:
            self.seen[e][tok[0]] = tok[1]
            self.q[e].append(([tok], None, None))

    def barrier(self):
        toks = [(e, c) for e, c in self.cnt.items() if c > 0] + [(k, c) for k, c in self.dcnt.items() if c > 0]
        for e in self.ENG:
            for tok in toks:
                if tok[0] != e:
                    self.wait_tok(e, tok)

    def emit(self, block):
        def mk(e):
            def body(engine):
                for waits, fn, inc in items:
                    for k, v in waits:
                        engine.wait_ge(self._semobj(k), v)
                    if fn is not None:
                        fn(engine).then_inc(self._semobj(inc[0]), inc[1])
            items = self.q[e]
            self.q[e] = []
            return body
        block.tensor(mk('pe'))
        block.vector(mk('dve'))
        block.scalar(mk('act'))
        block.gpsimd(mk('pool'))
        block.sync(mk('sp'))


def host_consts():
    c = {}
    inv_ret = 10000.0 ** (-np.arange(0, 64, 2, dtype=np.float32) / 64)
    inv_dsa = 500000.0 ** (-np.arange(0, 16, 2, dtype=np.float32) / 16)
    inv_idx = 500000.0 ** (-np.arange(0, 8, 2, dtype=np.float32) / 8)
    invf = np.concatenate([inv_ret, inv_dsa, inv_idx]).astype(np.float32)
    c["c_invf"] = np.broadcast_to(invf[None, :], (128, 44)).copy()
    log_g = np.log1p(-np.exp2(-5.0 - np.arange(8, dtype=np.float64)))
    m = np.arange(128)[:, None]
    n = np.arange(128)[None, :]
    valid = (m // 64) <= (n // 64)
    maskT = np.zeros((128, 8, 128), np.float64)
    for h in range(8):
        maskT[:, h, :] = np.exp(log_g[h] * np.abs(n - m)) * valid * (64 ** -0.5)
    c["c_maskT"] = maskT.reshape(128, 1024).astype(np.float32)
    xiT = np.zeros((128, 4, 128), np.float64)
    dec = np.zeros((128, 4, 64), np.float64)
    for p in range(4):
        for e in range(2):
            h = 2 * p + e
            xiT[e * 64:(e + 1) * 64, p, :] = (np.exp(log_g[h] * (np.arange(128) + 1.0)) * (64 ** -0.5))[None, :]
            dec[e * 64:(e + 1) * 64, p, :] = np.exp(log_g[h] * 128.0)
    c["c_xiT"] = xiT.reshape(128, 512).astype(np.float32)
    c["c_decay"] = dec.reshape(128, 256).astype(np.float32)
    zeta = np.exp(log_g[None, :] * (127.0 - np.arange(128)[:, None]))
    c["c_zeta"] = zeta.astype(np.float32)
    t = np.arange(128)[:, None]
    s = np.arange(128)[None, :]
    c["c_cb"] = np.where((s // 64) <= (t // 64), 0.0, -1e30).astype(np.float32)
    c["c_ident"] = np.eye(128, dtype=np.float32)
    c["c_pow2"] = np.broadcast_to((2.0 ** -np.arange(N_BIS + 1, dtype=np.float64))[None, :], (128, N_BIS + 1)).astype(np.float32).copy()
    ipe = np.zeros((16, 128), np.float32)
    for i in range(16):
        ipe[i, i] = 1.0
        ipe[i, 64 + i] = 1.0
    c["c_ipe"] = ipe
    return c


CONST_SHAPES = {
    "c_invf": [128, 44], "c_maskT": [128, 1024], "c_xiT": [128, 512], "c_decay": [128, 256], "c_zeta": [128, 8],
    "c_cb": [128, 128], "c_ident": [128, 128], "c_pow2": [128, N_BIS + 1], "c_ipe": [16, 128],
}

COLMAP = [(0, 2560, 0), (2704, 3216, 2560), (2560, 2704, 3072), (3216, 3512, 3216)]


def build_nc(nseq, n_bis=N_BIS):
    import os
    STAGE = float(os.environ.get('K_STAGE', '99'))
    NTILES = int(os.environ.get('K_NTILES', str(NT)))
    SETUP = os.environ.get('K_SETUP', '123456')
    A_HEAD = int(os.environ.get('K_AHEAD', '-1'))
    XF = os.environ.get('K_XF', '12345')
    nc = bass.Bass("TRN2", target_bir_lowering=False, dynamic_dma_scratch_size=512)
    dt_ = {}

    def dram(name, shape, dt, kind="ExternalInput"):
        h = nc.dram_tensor(name, list(shape), dt, kind=kind)
        dt_[name] = h
        return h.ap()

    x_d = dram("x", [nseq, SEQ, DM], F32)
    p_d = dram("p", [nseq, SEQ, PLE], F32)
    pos_d = dram("positions", [SEQ], I32)
    nmix_d = dram("norm_mix", [DM], F32)
    win_d = dram("w_in", [DM, IN_W], F32)
    rnorm_d = dram("ret_norm", [512], F32)
    kvn_d = dram("kv_norm", [128], F32)
    wuk_d = dram("w_uk", [384, 128], F32)
    wuv_d = dram("w_uv", [8, 128, 64], F32)
    wout_d = dram("w_out", [DM, DM], F32)
    nple_d = dram("norm_ple", [DM], F32)
    wg_d = dram("w_ple_gate", [DM, DM], F32)
    wp_d = dram("w_ple_proj", [PLE, DM], F32)
    nfin_d = dram("norm_final", [DM], F32)
    cd = {k: dram(k, v, F32) for k, v in CONST_SHAPES.items()}
    out_d = dram("out", [nseq, SEQ, DM], F32, kind="ExternalOutput")

    with ExitStack() as es:
        S = Sched(nc, es)

        def sb(name, shape, dt, stack=es):
            return TL(stack.enter_context(nc.sbuf_tensor(name, list(shape), dt)), shape, name)

        def ps(name, shape, dt):
            return TL(es.enter_context(nc.psum_tensor(name, list(shape), dt)), shape, name)

        PW = [ps("pw0", [128, 512], F32), ps("pw1", [128, 512], F32)]
        P2 = ps("p2", [128, 8, 128], BF16)
        P3 = ps("p3", [128, 4, 128], F32)
        P4 = ps("p4", [128, 4, 128], F32)
        P2a = TL(None, [128, 4, 128], "p2a")
        P2b = TL(None, [128, 4, 128], "p2b")
        PQ = [ps("pq0", [128, 512], F32), ps("pq1", [128, 512], F32)]
        P7 = ps("p7", [128, 512], F32)
        pw_i = [0]

        def wide():
            pw_i[0] += 1
            return PW[pw_i[0] % 2]

        Win = sb("Win", [128, 8, IN_W], BF16)
        Wout = sb("Wout", [128, 8, DM], BF16)
        Wg = sb("Wg", [128, 8, DM], BF16)
        Wp = sb("Wp", [128, 2, DM], BF16)
        WukT = sb("WukT", [128, 8, 64], BF16)
        Wuv = sb("Wuv", [128, 8, 64], BF16)
        identw = sb("identw", [128, 128], F32)
        identb = sb("identb", [128, 128], BF16)
        I4 = sb("I4", [128, 4, 128], BF16)
        maskT = sb("maskT", [128, 8, 128], BF16)
        xiT = sb("xiT", [128, 4, 128], F32)
        decay = sb("decay", [128, 4, 64], F32)
        zeta = sb("zeta", [128, 8], F32)
        cb = sb("cb", [128, 128], F32)
        cbm = sb("cbm", [128, 128], BF16)
        pow2 = sb("pow2", [128, n_bis + 1], F32)
        ipe = sb("ipe", [16, 128], BF16)
        zt = sb("zt", [128, 264], BF16)
        gfin = sb("gfin", [128, DM], F32)
        gret = sb("gret", [128, 512], F32)
        COS = sb("COS", [128, NT, 44], F32)
        SIN = sb("SIN", [128, NT, 44], F32)
        mhalf = sb("mhalf", [128, 8], F32)
        mone = sb("mone", [128, 8], F32)
        gkv = sb("gkv", [128, 1], F32)

        KT = [sb("KT%d" % j, [128, 4, 128], BF16) for j in range(NT)]
        VX = [sb("VX%d" % j, [128, 8, 66], BF16) for j in range(NT)]
        KI = [TL(None, [128, 128], "KI%d" % j) for j in range(NT)]
        KIall = sb("KIall", [128, SEQ], BF16)
        st = sb("st", [128, 4, 64], F32)
        stb = sb("stb", [128, 4, 64], BF16)

        block = es.enter_context(nc.Block())

        with ExitStack() as ses:
            stg = [sb("stg%d" % i, [128, IN_W], F32, ses) for i in range(2)]
            stg2 = [sb("stgb%d" % i, [128, DM], F32, ses) for i in range(2)]
            cst = sb("cst", [16, 128], F32, ses)
            identf = sb("identf", [128, 128], F32, ses)
            gmix = sb("gmix", [128, 8], F32, ses)
            gple = sb("gple", [128, 8], F32, ses)
            wukr = sb("wukr", [128, 3, 128], F32, ses)
            wuvf = sb("wuvf", [128, 8, 64], F32, ses)
            posi = sb("posi", [128, NT], I32, ses)
            posf = sb("posf", [128, NT], F32, ses)
            invf = sb("invf", [128, 44], F32, ses)
            U = sb("U", [128, NT * 44], F32, ses)
            U1 = sb("U1", [128, NT * 44], F32, ses)
            U2 = sb("U2", [128, NT * 44], F32, ses)

            def ld(dst, src, key, **kw):
                S.dma('sp', lambda q, d=dst, s=src, kw=kw: q.dma_start(out=d, in_=s, **kw), key, writes=[])

            def load(tile_, dst_ap, src, key, **kw):
                return S.dma('sp', lambda q, d=dst_ap, s=src, kw=kw: q.dma_start(out=d, in_=s, **kw), key, writes=[tile_])

            load(identf, identf[:], cd["c_ident"][:, :], "c0")
            load(stg2[0], stg2[0][:], cd["c_maskT"][:, :], "c1")
            S.op('dve', lambda v: v.tensor_copy(out=maskT.ap(0, 128, 0, [[1, 1024]]), in_=stg2[0][:]), reads=[stg2[0]], writes=[maskT])
            load(xiT, xiT.ap(0, 128, 0, [[1, 512]]), cd["c_xiT"][:, :], "c2")
            load(decay, decay.ap(0, 128, 0, [[1, 256]]), cd["c_decay"][:, :], "c3")
            load(zeta, zeta[:], cd["c_zeta"][:, :], "c4")
            load(cb, cb[:], cd["c_cb"][:, :], "c5")
            load(pow2, pow2[:], cd["c_pow2"][:, :], "c6")
            load(cst, cst[:], cd["c_ipe"][:, :], "c7")
            load(invf, invf[:], cd["c_invf"][:, :], "c8")
            load(gmix, gmix[:], nmix_d.rearrange("(c p) -> p c", p=128), "c9", allow_slow_non_contiguous=True)
            load(gple, gple[:], nple_d.rearrange("(c p) -> p c", p=128), "c10", allow_slow_non_contiguous=True)
            load(gkv, gkv[:], kvn_d.rearrange("(c o) -> c o", o=1), "c11", allow_slow_non_contiguous=True)
            load(gfin, gfin[:], bass.AP(dt_["norm_final"], 0, [[0, 128], [1, DM]]), "c12")
            load(gret, gret[:], bass.AP(dt_["ret_norm"], 0, [[0, 128], [1, 512]]), "c13")
            load(posi, posi[:], pos_d.rearrange("(j p) -> p j", p=128), "c14", allow_slow_non_contiguous=True)
            load(wukr, wukr[:], wuk_d.rearrange("(a p) c -> p a c", p=128), "c15")
            load(wuvf, wuvf[:], wuv_d.rearrange("h c d -> c h d"), "c16")

            S.op('dve', lambda v: v.tensor_copy(out=identb[:], in_=identf[:]), reads=[identf], writes=[identb])
            S.op('dve', lambda v: v.tensor_copy(out=I4.ap(0, 128, 0, [[128, 4], [1, 128]]),
                                                in_=identf.ap(0, 128, 0, [[0, 4], [1, 128]])), reads=[identf], writes=[I4])
            cw = (8 ** -0.5) * (32 ** -0.5)
            S.op('dve', lambda v: v.tensor_scalar(out=identw[:], in0=identf[:], scalar1=cw, scalar2=None, op0=ALU.mult),
                 reads=[identf], writes=[identw])
            S.op('dve', lambda v: v.tensor_scalar(out=cbm[:], in0=cb[:], scalar1=NEG, scalar2=None, op0=ALU.max),
                 reads=[cb], writes=[cbm])
            S.op('dve', lambda v: v.tensor_copy(out=ipe[:], in_=cst[:]), reads=[cst], writes=[ipe])
            S.op('pool', lambda g: g.memset(zt[:], 0.0), writes=[zt])
            S.op('pool', lambda g: g.memset(mhalf[:], -0.5), writes=[mhalf])
            S.op('pool', lambda g: g.memset(mone[:], -1.0), writes=[mone])
            S.op('dve', lambda v: v.tensor_scalar(out=gret[:], in0=gret[:], scalar1=0.5, scalar2=None, op0=ALU.mult),
                 reads=[gret], writes=[gret])
            for j in range(NT):
                S.op('pool', lambda g, j=j: g.memset(VX[j][:], 1.0), writes=[VX[j]])

            for c in (range(8) if '2' in SETUP else []):
                sg_ = stg[c % 2]
                load(sg_, sg_[:], win_d[c * 128:(c + 1) * 128, :], "win%d" % (c % 2))
                for k, (s0, s1, d0) in enumerate(COLMAP):
                    if k == 0:
                        half = (s1 - s0) // 2
                        S.op('act', lambda a, sg_=sg_, c=c, s0=s0, d0=d0, half=half: a.activation(
                            out=Win[:, c, d0:d0 + half], in_=sg_[:, s0:s0 + half], func=AF.Identity, scale=gmix[:, c:c + 1]),
                            reads=[sg_, gmix], writes=[Win])
                        S.op('dve', lambda v, sg_=sg_, c=c, s0=s0 + half, s1=s1, d0=d0 + half: v.tensor_scalar(
                            out=Win[:, c, d0:d0 + (s1 - s0)], in0=sg_[:, s0:s1], scalar1=gmix[:, c:c + 1], scalar2=None,
                            op0=ALU.mult), reads=[sg_, gmix], writes=[Win])
                    else:
                        S.op('pool', lambda g, sg_=sg_, c=c, s0=s0, s1=s1, d0=d0: g.tensor_scalar(
                            out=Win[:, c, d0:d0 + (s1 - s0)], in0=sg_[:, s0:s1], scalar1=gmix[:, c:c + 1], scalar2=1.0,
                            op0=ALU.mult, op1=ALU.mult), reads=[sg_, gmix], writes=[Win])
            k2 = 0
            for c in (range(8) if '3' in SETUP else []):
                sg_ = stg2[k2 % 2]; k2 += 1
                load(sg_, sg_[:], wout_d[c * 128:(c + 1) * 128, :], "w2%d" % (k2 % 2))
                S.op('act', lambda a, sg_=sg_, c=c: a.copy(out=Wout[:, c, :], in_=sg_[:]), reads=[sg_], writes=[Wout])
            for c in (range(8) if '3' in SETUP else []):
                sg_ = stg2[k2 % 2]; k2 += 1
                load(sg_, sg_[:], wg_d[c * 128:(c + 1) * 128, :], "w2%d" % (k2 % 2))
                S.op('dve', lambda v, sg_=sg_, c=c: v.tensor_scalar(out=Wg[:, c, :], in0=sg_[:], scalar1=gple[:, c:c + 1],
                                                                    scalar2=None, op0=ALU.mult), reads=[sg_, gple], writes=[Wg])
            for c in (range(2) if '3' in SETUP else []):
                sg_ = stg2[k2 % 2]; k2 += 1
                load(sg_, sg_[:], wp_d[c * 128:(c + 1) * 128, :], "w2%d" % (k2 % 2))
                S.op('act', lambda a, sg_=sg_, c=c: a.activation(out=Wp[:, c, :], in_=sg_[:], func=AF.Copy, scale=0.5),
                     reads=[sg_], writes=[Wp])
            S.op('pool', lambda g: g.memset(WukT[:], 0.0), writes=[WukT])
            for a_ in (range(3) if '4' in SETUP else []):
                S.op('pe', lambda t, a_=a_: t.transpose(out=PW[0][:, a_ * 128:(a_ + 1) * 128], in_=wukr[:, a_, :],
                                                        identity=identf[:]), reads=[wukr, identf], writes=[PW[0]])
            if '4' in SETUP:
              S.op('dve', lambda v: v.tensor_scalar(out=WukT.ap(0, 128, 16, [[64, 8], [1, 48]]),
                                                  in0=PW[0].ap(0, 128, 0, [[48, 8], [1, 48]]),
                                                  scalar1=gkv[:, 0:1], scalar2=0.125, op0=ALU.mult, op1=ALU.mult),
                 reads=[PW[0], gkv, WukT], writes=[WukT])
            if '5' in SETUP:
              S.op('dve', lambda v: v.tensor_scalar(out=Wuv.ap(0, 128, 0, [[1, 512]]), in0=wuvf.ap(0, 128, 0, [[1, 512]]),
                                                  scalar1=gkv[:, 0:1], scalar2=0.5, op0=ALU.mult, op1=ALU.mult),
                 reads=[wuvf, gkv], writes=[Wuv])
            S.op('dve', lambda v: v.tensor_copy(out=posf[:], in_=posi[:]), reads=[posi], writes=[posf])
            for j in range(NT):
                S.op('dve', lambda v, j=j: v.tensor_scalar(out=U[:, j * 44:(j + 1) * 44], in0=invf[:],
                                                           scalar1=posf[:, j:j + 1], scalar2=1.0 / TWO_PI,
                                                           op0=ALU.mult, op1=ALU.mult), reads=[invf, posf], writes=[U])
            flat = lambda t: t.ap(0, 128, 0, [[1, NT * 44]])
            for kind in (("sin", "cos") if '6' in SETUP else ()):
                if kind == "cos":
                    S.op('dve', lambda v: v.tensor_scalar(out=U[:], in0=U[:], scalar1=0.25, scalar2=None, op0=ALU.add),
                         reads=[U], writes=[U])
                S.op('dve', lambda v: v.tensor_scalar(out=U1[:], in0=U[:], scalar1=MAGIC, scalar2=None, op0=ALU.add),
                     reads=[U], writes=[U1])
                S.op('dve', lambda v: v.tensor_scalar(out=U2[:], in0=U1[:], scalar1=-MAGIC, scalar2=None, op0=ALU.add),
                     reads=[U1], writes=[U2])
                S.op('dve', lambda v: v.tensor_tensor(out=U1[:], in0=U[:], in1=U2[:], op=ALU.subtract),
                     reads=[U, U2, U1], writes=[U1])
                dst = SIN if kind == "sin" else COS
                S.op('act', lambda a, dst=dst: a.activation(out=flat(dst), in_=U1[:], func=AF.Sin, scale=TWO_PI * (1 - 1e-6)),
                     reads=[U1], writes=[dst])
            S.barrier()
            S.emit(block)

        ssv = sb("ssv", [128, 8], F32)
        rsv = sb("rsv", [128, 8], F32)
        hbf = sb("hbf", [128, DM], BF16)
        hT = sb("hT", [128, 8, 128], BF16)
        rqf = sb("rqf", [128, 512], F32)
        tmpA = sb("tmpA", [128, 512], F32)
        tmpB = sb("tmpB", [128, 512], F32)
        rqb = sb("rqb", [128, 512], BF16)
        rkb = sb("rkb", [128, 512], BF16)
        qT = sb("qT", [128, 4, 128], BF16)
        qxT = sb("qxT", [128, 4, 128], BF16)
        kT = sb("kT", [128, 4, 128], BF16)
        kz = sb("kz", [128, 512], BF16)
        vb = sb("vb", [128, 512], BF16)
        thr_ = tmpA
        sgr = sb("sgr", [128, 512], BF16)
        qdb = rkb
        dqf = sb("dqf", [128, 8, 16], F32)
        dqa = tmpA
        dqb = tmpB
        sm = sb("sm", [128, 440], F32)
        smA = sb("smA", [128, 64], F32)
        smB = sb("smB", [128, 64], F32)
        kpf = sb("kpf", [128, 16], F32)
        nb = sb("nb", [128, 128], BF16)
        kpeb = sb("kpeb", [128, 16], BF16)
        ik4 = sb("ik4", [128, 4, 32], BF16)
        iqb = sb("iqb", [128, 256], BF16)
        diag = sb("diag", [128, 8, 128], BF16)
        nT = sb("nT", [128, 128], BF16)
        kpeT = sb("kpeT", [16, 128], BF16)
        qiT = sb("qiT", [128, 3, 128], BF16)
        PT = [sb("PT%d" % i, [128, 128], BF16) for i in range(4)]
        PTf = [sb("PTf%d" % i, [128, 128], BF16) for i in range(4)]
        of_ = rqf
        osq = tmpA
        gs1 = sb("gs1", [128, 8], F32)
        gs2 = sb("gs2", [128, 8], F32)
        gmean = sb("gmean", [128, 8], F32)
        gvar = sb("gvar", [128, 8], F32)
        grstd = sb("grstd", [128, 8], F32)
        retb = rqb
        Rb = [sb("Rb%d" % i, [128, 512], BF16) for i in range(2)]
        XT = [sb("xt%d" % i, [128, DM], F32) for i in range(2)]
        PTL = [sb("pt%d" % i, [128, PLE], F32) for i in range(2)]
        QTs = [sb("QT%d" % i, [128, 8, 128], BF16) for i in range(2)]
        SGD = [sb("sgd%d" % i, [128, 512], BF16) for i in range(2)]
        MIXT = [sb("mixT%d" % i, [128, 8, 128], BF16) for i in range(2)]
        SC = sb("SC", [128, SEQ], F32)
        amp = sb("amp", [128, 8], F32)
        MB = sb("MB", [128, SEQ], BF16)
        DL = sb("DL", [128, n_bis + 1], F32)
        mids = sb("mids", [128, n_bis + 2], F32)
        cnts = sb("cnts", [128, n_bis + 1], F32)
        sgs = sb("sgs", [128, n_bis + 1], F32)
        Eb = [sb("Eb%d" % i, [128, 512], BF16) for i in range(2)]
        rec = sb("rec", [128, 4], F32)
        dsab = sb("dsab", [128, 512], BF16)
        ssvB = sb("ssvB", [128, 8], F32)
        rsvB = sb("rsvB", [128, 8], F32)
        hbf2 = sb("hbf2", [128, DM], BF16)
        h2T = sb("h2T", [128, 8, 128], BF16)
        th2 = sb("th2", [128, 512], F32)
        pbf = sb("pbf", [128, PLE], BF16)
        pT = sb("pT", [128, 2, 128], BF16)

        def rms_rstd(src_ap, n, col, reads, junk, ssv_, rsv_):
            S.op('act', lambda a: a.activation(out=junk[:, 0:n], in_=src_ap, func=AF.Square, accum_out=ssv_[:, col:col + 1]),
                 reads=reads, writes=[junk, ssv_])
            S.op('pool', lambda g: g.tensor_scalar(out=ssv_[:, col:col + 1], in0=ssv_[:, col:col + 1], scalar1=1.0 / n,
                                                   scalar2=EPS, op0=ALU.mult, op1=ALU.add), reads=[ssv_], writes=[ssv_])
            S.op('pool', lambda g: g.tensor_tensor(out=rsv_[:, col:col + 1], in0=ssv_[:, col:col + 1], in1=mhalf[:, 0:1],
                                                   op=ALU.pow), reads=[ssv_, mhalf], writes=[rsv_])

        def transposes(src_list, dst_ap_fn, reads, writes, base=0):
            pb = P2a if base == 0 else P2b
            for k, a_in in enumerate(src_list):
                S.op('pe', lambda t, k=k, a_in=a_in: t.transpose(out=P2[:, base + k, :], in_=a_in, identity=identb[:]),
                     reads=reads + [identb], writes=[pb] if len(src_list) <= 4 else [P2a, P2b])
            n = len(src_list)
            S.op('act', lambda a: a.copy(out=dst_ap_fn(), in_=P2[:, base:base + n, :]),
                 reads=[pb] if n <= 4 else [P2a, P2b], writes=writes)

        def V4(t, off, hs, nh, half):
            return bass.AP(t.t, off, [[t.row, 128], [hs, nh], [half, 2], [1, half]])

        def V3(t, off, hs, nh, half):
            return bass.AP(t.t, off, [[t.row, 128], [hs, nh], [1, half]])

        def rotary(eng, src, soff, shs, dst, doff, dhs, ta, tb, nh, half, tcol, j):
            def tb4(t):
                return bass.AP(t.t, j * 44 + tcol, [[NT * 44, 128], [0, nh], [0, 2], [1, half]])

            def tb3(t):
                return bass.AP(t.t, j * 44 + tcol, [[NT * 44, 128], [0, nh], [1, half]])
            ths = 2 * half
            S.op(eng, lambda g: g.tensor_tensor(out=V4(ta, 0, ths, nh, half), in0=V4(src, soff, shs, nh, half),
                                                in1=tb4(COS), op=ALU.mult), reads=[src, COS], writes=[ta])
            S.op(eng, lambda g: g.tensor_tensor(out=V3(tb, 0, ths, nh, half), in0=V3(src, soff + half, shs, nh, half),
                                                in1=tb3(SIN), op=ALU.mult), reads=[src, SIN], writes=[tb])
            S.op(eng, lambda g: g.tensor_tensor(out=V3(tb, half, ths, nh, half), in0=V3(src, soff, shs, nh, half),
                                                in1=tb3(SIN), op=ALU.mult), reads=[src, SIN, tb], writes=[tb])
            S.op(eng, lambda g: g.tensor_tensor(out=V3(dst, doff, dhs, nh, half), in0=V3(ta, 0, ths, nh, half),
                                                in1=V3(tb, 0, ths, nh, half), op=ALU.subtract),
                 reads=[ta, tb, dst], writes=[dst])
            S.op(eng, lambda g: g.tensor_tensor(out=V3(dst, doff + half, dhs, nh, half), in0=V3(ta, half, ths, nh, half),
                                                in1=V3(tb, half, ths, nh, half), op=ALU.add),
                 reads=[ta, tb, dst], writes=[dst])

        unit = [0]
        out_toks = []
        tdone = [-1]
        for i_ in range(2):
            S.op('pool', lambda g, i_=i_: g.memset(QTs[i_][:], 0.0), writes=[QTs[i_]])

        def stage_A(q, j, k):
            xt = XT[k % 2]; pt = PTL[k % 2]; QT = QTs[k % 2]; sgd = SGD[k % 2]; mixT = MIXT[k % 2]
            r0 = j * 128
            n = 128 * (j + 1)
            if j == 0:
                S.op('pool', lambda g: g.memset(st[:], 0.0), writes=[st])
                S.op('pool', lambda g: g.memset(stb[:], 0.0), writes=[stb])
            S.dma('sp', lambda qq: qq.dma_start(out=xt[:], in_=x_d[q, r0:r0 + 128, :]), "ldx%d" % (k % 2), writes=[xt])
            S.dma('sp', lambda qq: qq.dma_start(out=pt[:], in_=p_d[q, r0:r0 + 128, :]), "ldp%d" % (k % 2), writes=[pt])
            rms_rstd(xt[:], DM, 0, [xt], hbf, ssv, rsv)
            S.op('act', lambda a: a.activation(out=hbf[:], in_=xt[:], func=AF.Identity, scale=rsv[:, 0:1]),
                 reads=[xt, rsv], writes=[hbf])
            transposes([hbf[:, c * 128:(c + 1) * 128] for c in range(8)], lambda: hT[:], [hbf], [hT])
            yield

            def proj(g, width):
                bank = wide()
                for c in range(8):
                    S.op('pe', lambda t, c=c, bank=bank: t.matmul(bank[:, 0:width], lhsT=hT[:, c, :],
                                                                  rhs=Win[:, c, g * 512:g * 512 + width],
                                                                  start=(c == 0), stop=(c == 7)),
                         reads=[hT, Win], writes=[bank])
                return bank

            bank = proj(6, 440)
            S.op('act', lambda a, bank=bank: a.copy(out=sm[:], in_=bank[:, 0:440]), reads=[bank], writes=[sm])
            rms_rstd(sm[:, 0:128], 128, 1, [sm], hbf, ssv, rsv)
            S.op('pool', lambda g: g.tensor_scalar(out=nb[:], in0=sm[:, 0:128], scalar1=rsv[:, 1:2], scalar2=1.0,
                                                   op0=ALU.mult, op1=ALU.mult), reads=[sm, rsv], writes=[nb])
            rotary('pool', sm, 128, 16, kpf, 0, 16, smA, smB, 1, 8, 32, j)
            S.op('pool', lambda g: g.tensor_scalar(out=kpeb[:], in0=kpf[:], scalar1=0.125, scalar2=1.0,
                                                   op0=ALU.mult, op1=ALU.mult), reads=[kpf], writes=[kpeb])
            rotary('pool', sm, 400, 8, sm, 400, 8, smA, smB, 1, 4, 40, j)
            S.op('pool', lambda g: g.tensor_copy(out=ik4[:], in_=sm.ap(0, 128, 400, [[0, 4], [1, 32]])),
                 reads=[sm], writes=[ik4])
            rotary('pool', sm, 144, 32, sm, 144, 32, smA, smB, 8, 4, 40, j)
            S.op('pool', lambda g: g.tensor_copy(out=iqb[:], in_=sm[:, 144:400]), reads=[sm], writes=[iqb])
            S.op('pool', lambda g: g.tensor_tensor(out=diag[:], in0=identw.ap(0, 128, 0, [[0, 8], [1, 128]]),
                                                   in1=sm.ap(0, 128, 432, [[1, 8], [0, 128]]), op=ALU.mult),
                 reads=[identw, sm], writes=[diag])
            yield
            S.op('pe', lambda t: t.transpose(out=P2[:, 0, :], in_=nb[:], identity=identb[:]), reads=[nb, identb], writes=[P2a])
            S.op('pe', lambda t: t.transpose(out=P2[0:16, 1, :], in_=kpeb[:], identity=identb[:]),
                 reads=[kpeb, identb], writes=[P2a])
            S.op('pe', lambda t: t.transpose(out=P2[:, 2, :], in_=ik4.ap(0, 128, 0, [[1, 128]]), identity=identb[:]),
                 reads=[ik4, identb], writes=[P2a])
            S.op('pe', lambda t: t.transpose(out=P2[0:96, 4, :], in_=iqb[:, 0:96], identity=identb[:]),
                 reads=[iqb, identb], writes=[P2b])
            S.op('pe', lambda t: t.transpose(out=P2[0:96, 5, :], in_=iqb[:, 96:192], identity=identb[:]),
                 reads=[iqb, identb], writes=[P2b])
            S.op('pe', lambda t: t.transpose(out=P2[0:64, 6, :], in_=iqb[:, 192:256], identity=identb[:]),
                 reads=[iqb, identb], writes=[P2b])
            S.op('act', lambda a: a.copy(out=nT[:], in_=P2[:, 0, :]), reads=[P2a], writes=[nT])
            S.op('act', lambda a: a.copy(out=kpeT[:], in_=P2[0:16, 1, :]), reads=[P2a], writes=[kpeT])
            S.op('act', lambda a: a.copy(out=KIall[:, j * 128:(j + 1) * 128], in_=P2[:, 2, :]), reads=[P2a], writes=[KI[j]])
            S.op('act', lambda a: a.copy(out=qiT[0:96, 0:2, :], in_=P2[0:96, 4:6, :]), reads=[P2b], writes=[qiT])
            S.op('act', lambda a: a.copy(out=qiT[0:64, 2, :], in_=P2[0:64, 6, :]), reads=[P2b], writes=[qiT])
            yield
            for p in range(4):
                S.op('pe', lambda t, p=p: t.matmul(P3[:, p, :], lhsT=WukT.ap(0, 128, p * 128, [[1, 128]]), rhs=nT[:],
                                                   start=True, stop=False), reads=[WukT, nT], writes=[P3])
                S.op('pe', lambda t, p=p: t.matmul(P3[:, p, :], lhsT=ipe[:], rhs=kpeT[:], start=False, stop=True),
                     reads=[ipe, kpeT], writes=[P3])
            S.op('act', lambda a: a.copy(out=KT[j][:], in_=P3[:]), reads=[P3], writes=[KT[j]])
            bank = wide()
            S.op('pe', lambda t, bank=bank: t.matmul(bank[:], lhsT=nT[:], rhs=Wuv.ap(0, 128, 0, [[1, 512]]),
                                                     start=True, stop=True), reads=[nT, Wuv], writes=[bank])
            S.op('act', lambda a, bank=bank: a.copy(out=VX[j].ap(0, 128, 0, [[66, 8], [1, 64]]),
                                                    in_=bank.ap(0, 128, 0, [[64, 8], [1, 64]])),
                 reads=[bank], writes=[VX[j]])
            yield
            for which in (0, 1):
                bank = proj(which, 512)
                S.op('act', lambda a, bank=bank: a.copy(out=rqf[:], in_=bank[:]), reads=[bank], writes=[rqf])
                yield
                dstb = rqb if which == 0 else rkb
                rotary('pool', rqf, 0, 64, dstb, 0, 64, tmpA, tmpB, 8, 32, 0, j)
                yield
                if which == 0:
                    transposes([rqb[:, c * 128:(c + 1) * 128] for c in range(4)], lambda: qT[:], [rqb], [qT])
                    S.op('pool', lambda g: g.tensor_tensor(out=qxT[:], in0=qT[:], in1=xiT[:], op=ALU.mult),
                         reads=[qT, xiT], writes=[qxT])
                else:
                    transposes([rkb[:, c * 128:(c + 1) * 128] for c in range(4)], lambda: kT[:], [rkb], [kT], base=4)
                    S.op('pool', lambda g: g.tensor_tensor(
                        out=kz.ap(0, 128, 0, [[64, 8], [1, 64]]), in0=rkb.ap(0, 128, 0, [[64, 8], [1, 64]]),
                        in1=zeta.ap(0, 128, 0, [[1, 8], [0, 64]]), op=ALU.mult), reads=[rkb, zeta], writes=[kz])
                yield
            bank = proj(2, 512)
            S.op('act', lambda a, bank=bank: a.copy(out=vb[:], in_=bank[:]), reads=[bank], writes=[vb])
            yield
            for (g, dst) in ((3, sgr), (5, sgd)):
                bank = proj(g, 512)
                S.op('act', lambda a, bank=bank: a.activation(out=thr_[:], in_=bank[:], func=AF.Tanh, scale=0.5),
                     reads=[bank], writes=[thr_])
                if '1' in XF:
                    S.op('act', lambda a, bank=bank: a.copy(out=rqf[:], in_=bank[:]), reads=[bank], writes=[rqf])
                    S.op('pool', lambda g_: g_.tensor_tensor(out=tmpB[:], in0=thr_[:], in1=rqf[:], op=ALU.mult),
                         reads=[thr_, rqf], writes=[tmpB])
                    S.op('pool', lambda g_, dst=dst: g_.tensor_tensor(out=dst[:], in0=tmpB[:], in1=rqf[:], op=ALU.add),
                         reads=[tmpB, rqf], writes=[dst])
                else:
                    S.op('dve', lambda v, bank=bank, dst=dst: v.scalar_tensor_tensor(
                        out=dst[:], in0=thr_[:], scalar=1.0, in1=bank[:], op0=ALU.add, op1=ALU.mult),
                        reads=[thr_, bank], writes=[dst])
                yield
            bank = proj(4, 512)
            S.op('act', lambda a, bank=bank: a.copy(out=qdb[:], in_=bank[:]), reads=[bank], writes=[qdb])
            S.op('act', lambda a, bank=bank: a.copy(out=dqf[:], in_=bank.ap(0, 128, 0, [[64, 8], [1, 16]])),
                 reads=[bank], writes=[dqf])
            rotary('pool', dqf, 0, 16, qdb, 0, 64, dqa, dqb, 8, 8, 32, j)
            for c_ in range(4):
                S.op('pe', lambda t, c_=c_: t.transpose(out=P2[:, c_, :], in_=qdb[:, c_ * 128:(c_ + 1) * 128], identity=identb[:]),
                     reads=[qdb, identb], writes=[P2a])
            S.op('act', lambda a: a.copy(out=QT.ap(0, 64, 0, [[256, 4], [1, 128]]), in_=P2[0:64, 0:4, :]), reads=[P2a], writes=[QT])
            S.op('act', lambda a: a.copy(out=QT.ap(64, 64, 128, [[256, 4], [1, 128]]), in_=P2[64:128, 0:4, :]), reads=[P2a], writes=[QT])
            yield
            obank = wide()
            for h in range(8):
                p, e = divmod(h, 2)
                rr = slice(e * 64, (e + 1) * 64)
                r4 = h % 4
                S.op('pe', lambda t, p=p, rr=rr, r4=r4: t.matmul(P4[:, r4, :], lhsT=kT[rr, p, :], rhs=qT[rr, p, :],
                                                                 start=True, stop=True), reads=[kT, qT], writes=[P4])
                if '2' in XF:
                    S.op('act', lambda a, r4=r4: a.copy(out=PTf[r4][:], in_=P4[:, r4, :]), reads=[P4], writes=[PTf[r4]])
                    S.op('pool', lambda g_, h=h, r4=r4: g_.tensor_tensor(out=PT[r4][:], in0=PTf[r4][:], in1=maskT[:, h, :],
                                                                         op=ALU.mult), reads=[PTf[r4], maskT], writes=[PT[r4]])
                else:
                    S.op('dve', lambda v, h=h, r4=r4: v.tensor_tensor(out=PT[r4][:], in0=P4[:, r4, :], in1=maskT[:, h, :],
                                                                      op=ALU.mult), reads=[P4, maskT], writes=[PT[r4]])
                S.op('pe', lambda t, h=h, r4=r4, obank=obank: t.matmul(obank[:, h * 64:(h + 1) * 64], lhsT=PT[r4][:],
                                                                      rhs=vb[:, h * 64:(h + 1) * 64], start=True, stop=False),
                     reads=[PT[r4], vb], writes=[obank])
                S.op('pe', lambda t, h=h, p=p, rr=rr, obank=obank: t.matmul(obank[:, h * 64:(h + 1) * 64], lhsT=qxT[rr, p, :],
                                                                           rhs=stb[rr, p, :], start=False, stop=True),
                     reads=[qxT, stb], writes=[obank])
                if h % 2 == 1:
                    yield
            for p in range(4):
                S.op('pe', lambda t, p=p: t.matmul(P3[:, p, :], lhsT=kz[:, p * 128:(p + 1) * 128], rhs=vb[:, p * 128:(p + 1) * 128],
                                                   start=True, stop=True), reads=[kz, vb], writes=[P3])
            S.op('pool', lambda g: g.tensor_tensor(out=st[:], in0=st[:], in1=decay[:], op=ALU.mult),
                 reads=[st, decay], writes=[st])
            if '3' in XF:
                S.op('act', lambda a: a.copy(out=tmpB.ap(0, 64, 0, [[64, 4], [1, 64]]), in_=P3[0:64, :, 0:64]), reads=[P3], writes=[tmpB])
                S.op('act', lambda a: a.copy(out=tmpB.ap(64, 64, 0, [[64, 4], [1, 64]]), in_=P3[64:128, :, 64:128]), reads=[P3], writes=[tmpB])
                S.op('pool', lambda g: g.tensor_tensor(out=st.ap(0, 128, 0, [[1, 256]]), in0=st.ap(0, 128, 0, [[1, 256]]),
                                                       in1=tmpB[:, 0:256], op=ALU.add), reads=[st, tmpB], writes=[st])
            else:
                S.op('dve', lambda v: v.tensor_tensor(out=st[0:64, :, :], in0=st[0:64, :, :], in1=P3[0:64, :, 0:64], op=ALU.add),
                     reads=[st, P3], writes=[st])
                S.op('dve', lambda v: v.tensor_tensor(out=st[64:128, :, :], in0=st[64:128, :, :], in1=P3[64:128, :, 64:128],
                                                      op=ALU.add), reads=[st, P3], writes=[st])
            S.op('pool', lambda g: g.tensor_copy(out=stb[:], in_=st[:]), reads=[st], writes=[stb])
            yield
            if '4' in XF:
                for h in range(8):
                    S.op('act', lambda a, obank=obank, h=h: a.activation(out=of_[:, h * 64:(h + 1) * 64], in_=obank[:, h * 64:(h + 1) * 64],
                                                                         func=AF.Identity, accum_out=gs1[:, h:h + 1]),
                         reads=[obank], writes=[of_, gs1])
                    S.op('act', lambda a, obank=obank, h=h: a.activation(out=osq[:, h * 64:(h + 1) * 64], in_=obank[:, h * 64:(h + 1) * 64],
                                                                         func=AF.Square, accum_out=gs2[:, h:h + 1]),
                         reads=[obank], writes=[osq, gs2])
                o3 = lambda t: t.ap(0, 128, 0, [[64, 8], [1, 64]])
                bc8 = lambda t: t.ap(0, 128, 0, [[1, 8], [0, 64]])
                S.op('pool', lambda g: g.tensor_scalar(out=gmean[:], in0=gs1[:], scalar1=1.0 / 64, scalar2=1.0, op0=ALU.mult, op1=ALU.mult),
                     reads=[gs1], writes=[gmean])
                S.op('pool', lambda g: g.tensor_tensor(out=gs1[:], in0=gmean[:], in1=gmean[:], op=ALU.mult), reads=[gmean, gs1], writes=[gs1])
                S.op('pool', lambda g: g.tensor_scalar(out=gs2[:], in0=gs2[:], scalar1=1.0 / 64, scalar2=EPS, op0=ALU.mult, op1=ALU.add),
                     reads=[gs2], writes=[gs2])
                S.op('pool', lambda g: g.tensor_tensor(out=gvar[:], in0=gs2[:], in1=gs1[:], op=ALU.subtract), reads=[gs1, gs2], writes=[gvar])
            else:
                S.op('act', lambda a, obank=obank: a.copy(out=of_[:], in_=obank[:]), reads=[obank], writes=[of_])
                S.op('act', lambda a, obank=obank: a.activation(out=osq[:], in_=obank[:], func=AF.Square), reads=[obank], writes=[osq])
                o3 = lambda t: t.ap(0, 128, 0, [[64, 8], [1, 64]])
                bc8 = lambda t: t.ap(0, 128, 0, [[1, 8], [0, 64]])
                S.op('dve', lambda v: v.tensor_reduce(out=gs1[:], in_=o3(of_), axis=AX.X, op=ALU.add), reads=[of_], writes=[gs1])
                S.op('dve', lambda v: v.tensor_reduce(out=gs2[:], in_=o3(osq), axis=AX.X, op=ALU.add), reads=[osq], writes=[gs2])
                S.op('dve', lambda v: v.tensor_scalar(out=gmean[:], in0=gs1[:], scalar1=1.0 / 64, scalar2=None, op0=ALU.mult),
                     reads=[gs1], writes=[gmean])
                S.op('dve', lambda v: v.tensor_tensor(out=gs1[:], in0=gmean[:], in1=gmean[:], op=ALU.mult), reads=[gmean, gs1], writes=[gs1])
                S.op('dve', lambda v: v.tensor_scalar(out=gs2[:], in0=gs2[:], scalar1=1.0 / 64, scalar2=EPS, op0=ALU.mult, op1=ALU.add),
                     reads=[gs2], writes=[gs2])
                S.op('dve', lambda v: v.tensor_tensor(out=gvar[:], in0=gs2[:], in1=gs1[:], op=ALU.subtract), reads=[gs1, gs2], writes=[gvar])
                S.op('dve', lambda v: v.tensor_scalar(out=gvar[:], in0=gvar[:], scalar1=1e-12, scalar2=None, op0=ALU.max),
                     reads=[gvar], writes=[gvar])
            S.op('pool', lambda g: g.tensor_tensor(out=grstd[:], in0=gvar[:], in1=mhalf[:], op=ALU.pow),
                 reads=[gvar, mhalf], writes=[grstd])
            yield
            S.op('pool', lambda g: g.tensor_tensor(out=o3(of_), in0=o3(of_), in1=bc8(gmean), op=ALU.subtract),
                 reads=[of_, gmean], writes=[of_])
            S.op('pool', lambda g: g.tensor_tensor(out=o3(of_), in0=o3(of_), in1=bc8(grstd), op=ALU.mult),
                 reads=[of_, grstd], writes=[of_])
            S.op('pool', lambda g: g.tensor_tensor(out=osq[:], in0=gret[:], in1=sgr[:], op=ALU.mult),
                 reads=[gret, sgr, osq], writes=[osq])
            S.op('pool', lambda g: g.tensor_tensor(out=retb[:], in0=of_[:], in1=osq[:], op=ALU.mult),
                 reads=[of_, osq], writes=[retb])
            transposes([retb[:, c * 128:(c + 1) * 128] for c in range(4)], lambda: mixT[:, 0:4, :], [retb], [mixT], base=4)
            yield
            while tdone[0] < k - 1:
                yield
            if j >= 2:
                ng = (n + 511) // 512
                for sgi in range(ng):
                    w = min(512, n - 512 * sgi)
                    c0 = sgi * 512
                    kbufs = [KI[t_] for t_ in range(c0 // 128, (c0 + w) // 128)]
                    for h in range(8):
                        c4, r4 = divmod(h, 3)
                        rr = slice(r4 * 32, (r4 + 1) * 32)
                        zb = wide()
                        rb = Rb[h % 2]
                        S.op('pe', lambda t, zb=zb, rr=rr, c4=c4, c0=c0, w=w: t.matmul(
                            zb[:, 0:w], lhsT=qiT[rr, c4, :], rhs=KIall[rr, c0:c0 + w], start=True, stop=True),
                            reads=[qiT] + kbufs, writes=[zb])
                        S.op('act', lambda a, zb=zb, rb=rb, w=w: a.activation(out=rb[:, 0:w], in_=zb[:, 0:w], func=AF.Relu),
                             reads=[zb], writes=[rb])
                        S.op('pe', lambda t, h=h, rb=rb, w=w: t.matmul(P4.ap(0, 128, 0, [[1, w]]), lhsT=diag[:, h, :], rhs=rb[:, 0:w],
                                                                       start=(h == 0), stop=(h == 7)),
                             reads=[diag, rb], writes=[P4])
                        if h % 2 == 1:
                            yield
                    S.op('act', lambda a, c0=c0, w=w: a.copy(out=SC[:, c0:c0 + w], in_=P4.ap(0, 128, 0, [[1, w]])), reads=[P4], writes=[SC])
                    if '5' not in XF:
                        S.op('dve', lambda v, sgi=sgi, w=w, c0=c0: v.tensor_reduce(out=amp[:, sgi:sgi + 1], in_=SC[:, c0:c0 + w], axis=AX.X,
                                                                                   op=ALU.max, apply_absolute_value=True),
                             reads=[SC], writes=[amp])
                if '5' not in XF:
                    S.op('pool', lambda g: g.tensor_tensor(out=SC[:, n - 128:n], in0=SC[:, n - 128:n], in1=cb[:], op=ALU.add),
                         reads=[SC, cb], writes=[SC])
                yield

        def stage_T(q, j, k):
            xt = XT[k % 2]; pt = PTL[k % 2]; QT = QTs[k % 2]; sgd = SGD[k % 2]; mixT = MIXT[k % 2]
            r0 = j * 128
            n = 128 * (j + 1)
            if j >= 2:
                ng = (n + 511) // 512
                if '5' in XF:
                    S.op('dve', lambda v: v.tensor_reduce(out=amp[:, 7:8], in_=SC[:, 0:n], axis=AX.X, op=ALU.max, apply_absolute_value=True),
                         reads=[SC], writes=[amp])
                    S.op('dve', lambda v: v.tensor_tensor(out=SC[:, n - 128:n], in0=SC[:, n - 128:n], in1=cb[:], op=ALU.add),
                         reads=[SC, cb], writes=[SC])
                else:
                    S.op('dve', lambda v: v.tensor_reduce(out=amp[:, 7:8], in_=amp[:, 0:ng], axis=AX.X, op=ALU.max),
                         reads=[amp], writes=[amp])
                S.op('dve', lambda v: v.tensor_scalar(out=DL[:], in0=pow2[:], scalar1=amp[:, 7:8], scalar2=None, op0=ALU.mult),
                     reads=[pow2, amp], writes=[DL])
                S.op('dve', lambda v: v.tensor_scalar(out=mids[:, 0:1], in0=pow2[:, 0:1], scalar1=0.0, scalar2=None, op0=ALU.mult),
                     reads=[pow2], writes=[mids])
                for i in range(n_bis):
                    S.op('dve', lambda v, i=i: v.tensor_scalar(out=MB[:, 0:n], in0=SC[:, 0:n], scalar1=mids[:, i:i + 1],
                                                               scalar2=None, op0=ALU.is_gt, op1=ALU.add,
                                                               accum_out=cnts[:, i:i + 1]),
                         reads=[SC, mids], writes=[MB, cnts])
                    S.op('dve', lambda v, i=i: v.tensor_scalar(out=sgs[:, i:i + 1], in0=cnts[:, i:i + 1], scalar1=256.0,
                                                               scalar2=0.5, op0=ALU.is_ge, op1=ALU.subtract),
                         reads=[cnts], writes=[sgs])
                    S.op('dve', lambda v, i=i: v.scalar_tensor_tensor(out=mids[:, i + 1:i + 2], in0=sgs[:, i:i + 1],
                                                                      scalar=DL[:, i:i + 1], in1=mids[:, i:i + 1],
                                                                      op0=ALU.mult, op1=ALU.add),
                         reads=[sgs, DL, mids], writes=[mids])
                    yield
                S.op('dve', lambda v: v.tensor_tensor(out=mids[:, n_bis + 1:n_bis + 2], in0=mids[:, n_bis:n_bis + 1],
                                                      in1=DL[:, n_bis:n_bis + 1], op=ALU.subtract),
                     reads=[mids, DL], writes=[mids])
                S.op('dve', lambda v: v.tensor_scalar(out=MB[:, 0:n], in0=SC[:, 0:n], scalar1=mids[:, n_bis + 1:n_bis + 2],
                                                      scalar2=NEG, op0=ALU.is_le, op1=ALU.mult),
                     reads=[SC, mids], writes=[MB])
            else:
                if j == 1:
                    S.op('pool', lambda g: g.memset(MB[:, 0:128], 0.0), writes=[MB])
                S.op('pool', lambda g: g.tensor_copy(out=MB[:, n - 128:n], in_=cbm[:]), reads=[cbm], writes=[MB])
            tdone[0] = k
            yield

        def stage_B(q, j, k):
            xt = XT[k % 2]; pt = PTL[k % 2]; QT = QTs[k % 2]; sgd = SGD[k % 2]; mixT = MIXT[k % 2]
            r0 = j * 128
            n = 128 * (j + 1)
            for hg in range(2):
                S.op('pe', lambda t: t.matmul(P7[:, 0:264], lhsT=zt[0:32, 0:128], rhs=zt[0:32, 0:264], start=True, stop=False),
                     reads=[zt], writes=[P7])
                for i in range(j + 1):
                    unit[0] += 1
                    qk = PQ[unit[0] % 2]
                    eb = Eb[unit[0] % 2]
                    S.op('pe', lambda t, qk=qk, i=i: t.matmul(qk[:], lhsT=MB[:, i * 128:(i + 1) * 128],
                                                              rhs=I4.ap(0, 128, 0, [[1, 512]]), start=True, stop=False),
                         reads=[MB, I4], writes=[qk])
                    for hh in range(4):
                        h = hg * 4 + hh
                        p = h // 2
                        S.op('pe', lambda t, qk=qk, hh=hh, p=p, h=h, i=i: t.matmul(
                            qk[:, hh * 128:(hh + 1) * 128], lhsT=KT[i][:, p, :], rhs=QT[:, h, :], start=False, stop=True),
                            reads=[KT[i], QT], writes=[qk])
                    S.op('act', lambda a, qk=qk, eb=eb: a.activation(out=eb[:], in_=qk[:], func=AF.Exp), reads=[qk], writes=[eb])
                    for hh in range(4):
                        h = hg * 4 + hh
                        S.op('pe', lambda t, eb=eb, hh=hh, h=h, i=i: t.matmul(
                            P7[:, hh * 66:hh * 66 + 66], lhsT=eb[:, hh * 128:(hh + 1) * 128], rhs=VX[i][:, h, :],
                            start=False, stop=(i == j)), reads=[eb, VX[i]], writes=[P7])
                    yield
                S.op('dve', lambda v: v.reciprocal(out=rec[:], in_=P7.ap(0, 128, 64, [[66, 4]])), reads=[P7], writes=[rec])
                for hh in range(4):
                    h = hg * 4 + hh
                    S.op('dve', lambda v, hh=hh, h=h: v.scalar_tensor_tensor(
                        out=dsab[:, h * 64:(h + 1) * 64], in0=P7[:, hh * 66:hh * 66 + 64], scalar=rec[:, hh:hh + 1],
                        in1=sgd[:, h * 64:(h + 1) * 64], op0=ALU.mult, op1=ALU.mult), reads=[P7, rec, sgd, dsab], writes=[dsab])
                yield
            transposes([dsab[:, c * 128:(c + 1) * 128] for c in range(4)], lambda: mixT[:, 4:8, :], [dsab], [mixT])
            yield
            for g in range(2):
                bank = wide()
                for c in range(8):
                    S.op('pe', lambda t, c=c, g=g, bank=bank: t.matmul(bank[:], lhsT=mixT[:, c, :],
                                                                      rhs=Wout[:, c, g * 512:(g + 1) * 512],
                                                                      start=(c == 0), stop=(c == 7)),
                         reads=[mixT, Wout], writes=[bank])
                S.op('dve', lambda v, g=g, bank=bank: v.tensor_tensor(out=xt[:, g * 512:(g + 1) * 512], in0=bank[:],
                                                                     in1=xt[:, g * 512:(g + 1) * 512], op=ALU.add),
                     reads=[bank, xt], writes=[xt])
                yield
            rms_rstd(xt[:], DM, 2, [xt], hbf2, ssvB, rsvB)
            S.op('act', lambda a: a.activation(out=hbf2[:], in_=xt[:], func=AF.Identity, scale=rsvB[:, 2:3]),
                 reads=[xt, rsvB], writes=[hbf2])
            transposes([hbf2[:, c * 128:(c + 1) * 128] for c in range(8)], lambda: h2T[:], [hbf2], [h2T])
            yield
            S.op('act', lambda a: a.copy(out=pbf[:], in_=pt[:]), reads=[pt], writes=[pbf])
            S.op('pe', lambda t: t.transpose(out=P2[:, 0, :], in_=pbf[:, 0:128], identity=identb[:]), reads=[pbf, identb], writes=[P2a])
            S.op('pe', lambda t: t.transpose(out=P2[:, 1, :], in_=pbf[:, 128:256], identity=identb[:]), reads=[pbf, identb], writes=[P2a])
            S.op('act', lambda a: a.copy(out=pT[:], in_=P2[:, 0:2, :]), reads=[P2a], writes=[pT])
            for g in range(2):
                gb = wide()
                for c in range(8):
                    S.op('pe', lambda t, c=c, g=g, gb=gb: t.matmul(gb[:], lhsT=h2T[:, c, :], rhs=Wg[:, c, g * 512:(g + 1) * 512],
                                                                  start=(c == 0), stop=(c == 7)), reads=[h2T, Wg], writes=[gb])
                S.op('act', lambda a, gb=gb: a.activation(out=th2[:], in_=gb[:], func=AF.Tanh, scale=0.5), reads=[gb], writes=[th2])
                pb = wide()
                for c in range(2):
                    S.op('pe', lambda t, c=c, g=g, pb=pb: t.matmul(pb[:], lhsT=pT[:, c, :], rhs=Wp[:, c, g * 512:(g + 1) * 512],
                                                                  start=(c == 0), stop=(c == 1)), reads=[pT, Wp], writes=[pb])
                S.op('dve', lambda v, pb=pb: v.scalar_tensor_tensor(out=th2[:], in0=th2[:], scalar=1.0, in1=pb[:],
                                                                    op0=ALU.add, op1=ALU.mult), reads=[th2, pb], writes=[th2])
                S.op('pool', lambda gg, g=g: gg.tensor_tensor(out=xt[:, g * 512:(g + 1) * 512], in0=xt[:, g * 512:(g + 1) * 512],
                                                              in1=th2[:], op=ALU.add), reads=[xt, th2], writes=[xt])
                yield
            rms_rstd(xt[:], DM, 3, [xt], hbf2, ssvB, rsvB)
            S.op('dve', lambda v: v.scalar_tensor_tensor(out=xt[:], in0=xt[:], scalar=rsvB[:, 3:4], in1=gfin[:],
                                                         op0=ALU.mult, op1=ALU.mult), reads=[xt, rsvB, gfin], writes=[xt])
            tok = S.dma('sp', lambda qq: qq.dma_start(out=out_d[q, r0:r0 + 128, :], in_=xt[:]), "sto%d" % (k % 2), reads=[xt])
            out_toks.append(tok)
            yield

        def run(gens):
            gens = list(gens)
            while gens:
                for g_ in list(gens):
                    try:
                        next(g_)
                    except StopIteration:
                        gens.remove(g_)

        def advance(g_, nsteps):
            for _ in range(nsteps):
                try:
                    next(g_)
                except StopIteration:
                    return False
            return True

        kk = 0
        for q in range(nseq):
            run([stage_A(q, 0, kk)])
            for j in range(NTILES):
                gb = stage_B(q, j, kk)
                if A_HEAD >= 0:
                    run([stage_T(q, j, kk)])
                    if j + 1 < NTILES:
                        ga = stage_A(q, j + 1, kk + 1)
                        if advance(ga, A_HEAD):
                            run([ga, gb])
                        else:
                            run([gb])
                    else:
                        run([gb])
                else:
                    gt = stage_T(q, j, kk)
                    def tb():
                        yield from gt
                        yield from gb
                    if j + 1 < NTILES:
                        run([stage_A(q, j + 1, kk + 1), tb()])
                    else:
                        run([tb()])
                kk += 1
        for tok in out_toks[-2:]:
            S.wait_tok('sp', tok)
        S.emit(block)
    return nc


def kernel(x, p, positions, norm_mix, w_in, ret_norm, kv_norm, w_uk, w_uv, w_out,
           norm_ple, w_ple_gate, w_ple_proj, norm_final, _nseq=None, _ncores=N_CORES):
    f = lambda a: np.ascontiguousarray(np.asarray(a, dtype=np.float32))
    x = f(x)
    p = f(p)[0]
    B = x.shape[0]
    nseq = B // _ncores
    consts = host_consts()
    shared = {
        "positions": np.ascontiguousarray(np.asarray(positions, dtype=np.int32)),
        "norm_mix": f(norm_mix)[0], "w_in": f(w_in)[0], "ret_norm": f(ret_norm)[0], "kv_norm": f(kv_norm)[0],
        "w_uk": f(w_uk)[0].reshape(384, 128), "w_uv": f(w_uv)[0], "w_out": f(w_out)[0], "norm_ple": f(norm_ple)[0],
        "w_ple_gate": f(w_ple_gate)[0], "w_ple_proj": f(w_ple_proj)[0], "norm_final": f(norm_final),
    }
    shared.update(consts)
    nc = build_nc(nseq)
    in_maps = []
    for c in range(_ncores):
        d = dict(shared)
        d["x"] = np.ascontiguousarray(x[c * nseq:(c + 1) * nseq])
        d["p"] = np.ascontiguousarray(p[c * nseq:(c + 1) * nseq])
        in_maps.append(d)
    res = run_bass_kernel_spmd(nc, in_maps, core_ids=list(range(_ncores)))
    out = np.concatenate([np.asarray(r["out"], dtype=np.float32) for r in res.results], axis=0)
    return out
```

```python
import math
from contextlib import ExitStack

import numpy as np
import concourse.bass as bass
import concourse.mybir as mybir
from concourse.bass_utils import run_bass_kernel_spmd

F32 = mybir.dt.float32
BF16 = mybir.dt.bfloat16
I32 = mybir.dt.int32
AF = mybir.ActivationFunctionType
ALU = mybir.AluOpType
AX = mybir.AxisListType

N_CORES = 8
BATCH = 32
SEQ = 2048
DM = 1024
NT = SEQ // 128
PLE = 256
EPS = 1e-6
IN_W = 3512
N_BIS = 22
NEG = -30000.0
MAGIC = 12582912.0
TWO_PI = 2.0 * math.pi


class Buf:
    def __init__(self, name):
        self.name = name
        self.w = None
        self.r = {}


class TL:
    def __init__(self, t, shape, name):
        self.t = t
        self.shape = list(shape)
        self.b = Buf(name)
        self.row = int(np.prod(shape[1:]))

    def ap(self, p0, pn, off, dims):
        return bass.AP(self.t, p0 * self.row + off, [[self.row, pn]] + [list(d) for d in dims])

    def __getitem__(self, k):
        return self.t[k]


class Sched:
    ENG = ('pe', 'dve', 'act', 'pool', 'sp')

    def __init__(self, nc, es):
        self.nc = nc
        self.es = es
        self.q = {e: [] for e in self.ENG}
        self.sem = {e: es.enter_context(nc.semaphore("s_" + e)) for e in self.ENG if e != 'sp'}
        self.cnt = {e: 0 for e in self.sem}
        self.seen = {e: {} for e in self.ENG}
        self.dsem = {}
        self.dcnt = {}

    def _semobj(self, key):
        return self.sem[key] if key in self.sem else self.dsem[key]

    def _deps(self, e, reads, writes):
        need = {}

        def add(tok, kind):
            if tok is None:
                return
            k, v = tok
            if k == e and (e == 'pe' or kind != 'raw'):
                return
            if need.get(k, 0) < v:
                need[k] = v
        for b in reads:
            add(b.w, 'raw')
        for b in writes:
            add(b.w, 'waw')
            for k, v in b.r.items():
                add((k, v), 'war')
        out = []
        for k, v in need.items():
            if self.seen[e].get(k, 0) < v:
                self.seen[e][k] = v
                out.append((k, v))
        return out

    def _mark(self, tok, key, reads, writes):
        for b in reads:
            if b.r.get(key, 0) < tok[1]:
                b.r[key] = tok[1]
        for b in writes:
            b.w = tok
            b.r = {}

    def op(self, e, fn, reads=(), writes=()):
        reads = [t.b for t in reads]
        writes = [t.b for t in writes]
        waits = self._deps(e, reads, writes)
        self.cnt[e] += 1
        tok = (e, self.cnt[e])
        self.q[e].append((waits, fn, (e, 1)))
        self._mark(tok, e, reads, writes)
        return tok

    def dma(self, e, fn, semkey, reads=(), writes=()):
        reads = [t.b for t in reads]
        writes = [t.b for t in writes]
        if semkey not in self.dsem:
            self.dsem[semkey] = self.es.enter_context(self.nc.semaphore("d_" + semkey))
            self.dcnt[semkey] = 0
        waits = self._deps(e, reads, writes)
        self.dcnt[semkey] += 16
        tok = (semkey, self.dcnt[semkey])
        self.q[e].append((waits, fn, (semkey, 16)))
        self._mark(tok, semkey, reads, writes)
        return tok

    def wait_tok(self, e, tok):
        if self.seen[e].get(tok[0], 0) < tok[1]:
            self.seen[e][tok[0]] = tok[1]
            self.q[e].append(([tok], None, None))

    def barrier(self):
        toks = [(e, c) for e, c in self.cnt.items() if c > 0] + [(k, c) for k, c in self.dcnt.items() if c > 0]
        for e in self.ENG:
            for tok in toks:
                if tok[0] != e:
                    self.wait_tok(e, tok)

    def emit(self, block):
        def mk(e):
            def body(engine):
                for waits, fn, inc in items:
                    for k, v in waits:
                        engine.wait_ge(self._semobj(k), v)
                    if fn is not None:
                        fn(engine).then_inc(self._semobj(inc[0]), inc[1])
            items = self.q[e]
            self.q[e] = []
            return body
        block.tensor(mk('pe'))
        block.vector(mk('dve'))
        block.scalar(mk('act'))
        block.gpsimd(mk('pool'))
        block.sync(mk('sp'))


def host_consts():
    c = {}
    inv_ret = 10000.0 ** (-np.arange(0, 64, 2, dtype=np.float32) / 64)
    inv_dsa = 500000.0 ** (-np.arange(0, 16, 2, dtype=np.float32) / 16)
    inv_idx = 500000.0 ** (-np.arange(0, 8, 2, dtype=np.float32) / 8)
    invf = np.concatenate([inv_ret, inv_dsa, inv_idx]).astype(np.float32)
    c["c_invf"] = np.broadcast_to(invf[None, :], (128, 44)).copy()
    log_g = np.log1p(-np.exp2(-5.0 - np.arange(8, dtype=np.float64)))
    m = np.arange(128)[:, None]
    n = np.arange(128)[None, :]
    valid = (m // 64) <= (n // 64)
    maskT = np.zeros((128, 8, 128), np.float64)
    for h in range(8):
        maskT[:, h, :] = np.exp(log_g[h] * np.abs(n - m)) * valid * (64 ** -0.5)
    c["c_maskT"] = maskT.reshape(128, 1024).astype(np.float32)
    xiT = np.zeros((128, 4, 128), np.float64)
    dec = np.zeros((128, 4, 64), np.float64)
    for p in range(4):
        for e in range(2):
            h = 2 * p + e
            xiT[e * 64:(e + 1) * 64, p, :] = (np.exp(log_g[h] * (np.arange(128) + 1.0)) * (64 ** -0.5))[None, :]
            dec[e * 64:(e + 1) * 64, p, :] = np.exp(log_g[h] * 128.0)
    c["c_xiT"] = xiT.reshape(128, 512).astype(np.float32)
    c["c_decay"] = dec.reshape(128, 256).astype(np.float32)
    zeta = np.exp(log_g[None, :] * (127.0 - np.arange(128)[:, None]))
    c["c_zeta"] = zeta.astype(np.float32)
    t = np.arange(128)[:, None]
    s = np.arange(128)[None, :]
    c["c_cb"] = np.where((s // 64) <= (t // 64), 0.0, -1e30).astype(np.float32)
    c["c_ident"] = np.eye(128, dtype=np.float32)
    c["c_pow2"] = np.broadcast_to((2.0 ** -np.arange(N_BIS + 1, dtype=np.float64))[None, :], (128, N_BIS + 1)).astype(np.float32).copy()
    ipe = np.zeros((16, 128), np.float32)
    for i in range(16):
        ipe[i, i] = 1.0
        ipe[i, 64 + i] = 1.0
    c["c_ipe"] = ipe
    return c


CONST_SHAPES = {
    "c_invf": [128, 44], "c_maskT": [128, 1024], "c_xiT": [128, 512], "c_decay": [128, 256], "c_zeta": [128, 8],
    "c_cb": [128, 128], "c_ident": [128, 128], "c_pow2": [128, N_BIS + 1], "c_ipe": [16, 128],
}

COLMAP = [(0, 2560, 0), (2704, 3216, 2560), (2560, 2704, 3072), (3216, 3512, 3216)]


def build_nc(nseq, n_bis=N_BIS):
    import os
    STAGE = float(os.environ.get('K_STAGE', '99'))
    NTILES = int(os.environ.get('K_NTILES', str(NT)))
    SETUP = os.environ.get('K_SETUP', '123456')
    A_HEAD = int(os.environ.get('K_AHEAD', '-1'))
    XF = os.environ.get('K_XF', '12345')
    nc = bass.Bass("TRN2", target_bir_lowering=False, dynamic_dma_scratch_size=512)
    dt_ = {}

    def dram(name, shape, dt, kind="ExternalInput"):
        h = nc.dram_tensor(name, list(shape), dt, kind=kind)
        dt_[name] = h
        return h.ap()

    x_d = dram("x", [nseq, SEQ, DM], F32)
    p_d = dram("p", [nseq, SEQ, PLE], F32)
    pos_d = dram("positions", [SEQ], I32)
    nmix_d = dram("norm_mix", [DM], F32)
    win_d = dram("w_in", [DM, IN_W], F32)
    rnorm_d = dram("ret_norm", [512], F32)
    kvn_d = dram("kv_norm", [128], F32)
    wuk_d = dram("w_uk", [384, 128], F32)
    wuv_d = dram("w_uv", [8, 128, 64], F32)
    wout_d = dram("w_out", [DM, DM], F32)
    nple_d = dram("norm_ple", [DM], F32)
    wg_d = dram("w_ple_gate", [DM, DM], F32)
    wp_d = dram("w_ple_proj", [PLE, DM], F32)
    nfin_d = dram("norm_final", [DM], F32)
    cd = {k: dram(k, v, F32) for k, v in CONST_SHAPES.items()}
    out_d = dram("out", [nseq, SEQ, DM], F32, kind="ExternalOutput")

    with ExitStack() as es:
        S = Sched(nc, es)

        def sb(name, shape, dt, stack=es):
            return TL(stack.enter_context(nc.sbuf_tensor(name, list(shape), dt)), shape, name)

        def ps(name, shape, dt):
            return TL(es.enter_context(nc.psum_tensor(name, list(shape), dt)), shape, name)

        PW = [ps("pw0", [128, 512], F32), ps("pw1", [128, 512], F32)]
        P2 = ps("p2", [128, 8, 128], BF16)
        P3 = ps("p3", [128, 4, 128], F32)
        P4 = ps("p4", [128, 4, 128], F32)
        P2a = TL(None, [128, 4, 128], "p2a")
        P2b = P2a
        PQ = [ps("pq0", [128, 512], F32), ps("pq1", [128, 512], F32)]
        P7 = ps("p7", [128, 512], F32)
        pw_i = [0]

        def wide():
            pw_i[0] += 1
            return PW[pw_i[0] % 2]

        Win = sb("Win", [128, 8, IN_W], BF16)
        Wout = sb("Wout", [128, 8, DM], BF16)
        Wg = sb("Wg", [128, 8, DM], BF16)
        Wp = sb("Wp", [128, 2, DM], BF16)
        WukT = sb("WukT", [128, 8, 64], BF16)
        Wuv = sb("Wuv", [128, 8, 64], BF16)
        identw = sb("identw", [128, 128], F32)
        identb = sb("identb", [128, 128], BF16)
        I4 = sb("I4", [128, 4, 128], BF16)
        maskT = sb("maskT", [128, 8, 128], BF16)
        xiT = sb("xiT", [128, 4, 128], F32)
        decay = sb("decay", [128, 4, 64], F32)
        zeta = sb("zeta", [128, 8], F32)
        cb = sb("cb", [128, 128], F32)
        cbm = sb("cbm", [128, 128], BF16)
        pow2 = sb("pow2", [128, n_bis + 1], F32)
        ipe = sb("ipe", [16, 128], BF16)
        zt = sb("zt", [128, 264], BF16)
        gfin = sb("gfin", [128, DM], F32)
        gret = sb("gret", [128, 512], F32)
        COS = sb("COS", [128, NT, 44], F32)
        SIN = sb("SIN", [128, NT, 44], F32)
        mhalf = sb("mhalf", [128, 8], F32)
        mone = sb("mone", [128, 8], F32)
        gkv = sb("gkv", [128, 1], F32)

        KT = [sb("KT%d" % j, [128, 4, 128], BF16) for j in range(NT)]
        VX = [sb("VX%d" % j, [128, 8, 66], BF16) for j in range(NT)]
        KI = [TL(None, [128, 128], "KI%d" % j) for j in range(NT)]
        KIall = sb("KIall", [128, SEQ], BF16)
        st = sb("st", [128, 4, 64], F32)
        stb = sb("stb", [128, 4, 64], BF16)

        block = es.enter_context(nc.Block())

        with ExitStack() as ses:
            stg = [sb("stg%d" % i, [128, IN_W], F32, ses) for i in range(2)]
            stg2 = [sb("stgb%d" % i, [128, DM], F32, ses) for i in range(2)]
            cst = sb("cst", [16, 128], F32, ses)
            identf = sb("identf", [128, 128], F32, ses)
            gmix = sb("gmix", [128, 8], F32, ses)
            gple = sb("gple", [128, 8], F32, ses)
            wukr = sb("wukr", [128, 3, 128], F32, ses)
            wuvf = sb("wuvf", [128, 8, 64], F32, ses)
            posi = sb("posi", [128, NT], I32, ses)
            posf = sb("posf", [128, NT], F32, ses)
            invf = sb("invf", [128, 44], F32, ses)
            U = sb("U", [128, NT * 44], F32, ses)
            U1 = sb("U1", [128, NT * 44], F32, ses)
            U2 = sb("U2", [128, NT * 44], F32, ses)

            def ld(dst, src, key, **kw):
                S.dma('sp', lambda q, d=dst, s=src, kw=kw: q.dma_start(out=d, in_=s, **kw), key, writes=[])

            def load(tile_, dst_ap, src, key, **kw):
                return S.dma('sp', lambda q, d=dst_ap, s=src, kw=kw: q.dma_start(out=d, in_=s, **kw), key, writes=[tile_])

            load(identf, identf[:], cd["c_ident"][:, :], "c0")
            load(stg2[0], stg2[0][:], cd["c_maskT"][:, :], "c1")
            S.op('dve', lambda v: v.tensor_copy(out=maskT.ap(0, 128, 0, [[1, 1024]]), in_=stg2[0][:]), reads=[stg2[0]], writes=[maskT])
            load(xiT, xiT.ap(0, 128, 0, [[1, 512]]), cd["c_xiT"][:, :], "c2")
            load(decay, decay.ap(0, 128, 0, [[1, 256]]), cd["c_decay"][:, :], "c3")
            load(zeta, zeta[:], cd["c_zeta"][:, :], "c4")
            load(cb, cb[:], cd["c_cb"][:, :], "c5")
            load(pow2, pow2[:], cd["c_pow2"][:, :], "c6")
            load(cst, cst[:], cd["c_ipe"][:, :], "c7")
            load(invf, invf[:], cd["c_invf"][:, :], "c8")
            load(gmix, gmix[:], nmix_d.rearrange("(c p) -> p c", p=128), "c9", allow_slow_non_contiguous=True)
            load(gple, gple[:], nple_d.rearrange("(c p) -> p c", p=128), "c10", allow_slow_non_contiguous=True)
            load(gkv, gkv[:], kvn_d.rearrange("(c o) -> c o", o=1), "c11", allow_slow_non_contiguous=True)
            load(gfin, gfin[:], bass.AP(dt_["norm_final"], 0, [[0, 128], [1, DM]]), "c12")
            load(gret, gret[:], bass.AP(dt_["ret_norm"], 0, [[0, 128], [1, 512]]), "c13")
            load(posi, posi[:], pos_d.rearrange("(j p) -> p j", p=128), "c14", allow_slow_non_contiguous=True)
            load(wukr, wukr[:], wuk_d.rearrange("(a p) c -> p a c", p=128), "c15")
            load(wuvf, wuvf[:], wuv_d.rearrange("h c d -> c h d"), "c16")

            S.op('dve', lambda v: v.tensor_copy(out=identb[:], in_=identf[:]), reads=[identf], writes=[identb])
            S.op('dve', lambda v: v.tensor_copy(out=I4.ap(0, 128, 0, [[128, 4], [1, 128]]),
                                                in_=identf.ap(0, 128, 0, [[0, 4], [1, 128]])), reads=[identf], writes=[I4])
            cw = (8 ** -0.5) * (32 ** -0.5)
            S.op('dve', lambda v: v.tensor_scalar(out=identw[:], in0=identf[:], scalar1=cw, scalar2=None, op0=ALU.mult),
                 reads=[identf], writes=[identw])
            S.op('dve', lambda v: v.tensor_scalar(out=cbm[:], in0=cb[:], scalar1=NEG, scalar2=None, op0=ALU.max),
                 reads=[cb], writes=[cbm])
            S.op('dve', lambda v: v.tensor_copy(out=ipe[:], in_=cst[:]), reads=[cst], writes=[ipe])
            S.op('pool', lambda g: g.memset(zt[:], 0.0), writes=[zt])
            S.op('pool', lambda g: g.memset(mhalf[:], -0.5), writes=[mhalf])
            S.op('pool', lambda g: g.memset(mone[:], -1.0), writes=[mone])
            S.op('dve', lambda v: v.tensor_scalar(out=gret[:], in0=gret[:], scalar1=0.5, scalar2=None, op0=ALU.mult),
                 reads=[gret], writes=[gret])
            for j in range(NT):
                S.op('pool', lambda g, j=j: g.memset(VX[j][:], 1.0), writes=[VX[j]])

            for c in (range(8) if '2' in SETUP else []):
                sg_ = stg[c % 2]
                load(sg_, sg_[:], win_d[c * 128:(c + 1) * 128, :], "win%d" % (c % 2))
                for k, (s0, s1, d0) in enumerate(COLMAP):
                    if k == 0:
                        half = (s1 - s0) // 2
                        S.op('act', lambda a, sg_=sg_, c=c, s0=s0, d0=d0, half=half: a.activation(
                            out=Win[:, c, d0:d0 + half], in_=sg_[:, s0:s0 + half], func=AF.Identity, scale=gmix[:, c:c + 1]),
                            reads=[sg_, gmix], writes=[Win])
                        S.op('dve', lambda v, sg_=sg_, c=c, s0=s0 + half, s1=s1, d0=d0 + half: v.tensor_scalar(
                            out=Win[:, c, d0:d0 + (s1 - s0)], in0=sg_[:, s0:s1], scalar1=gmix[:, c:c + 1], scalar2=None,
                            op0=ALU.mult), reads=[sg_, gmix], writes=[Win])
                    else:
                        S.op('pool', lambda g, sg_=sg_, c=c, s0=s0, s1=s1, d0=d0: g.tensor_scalar(
                            out=Win[:, c, d0:d0 + (s1 - s0)], in0=sg_[:, s0:s1], scalar1=gmix[:, c:c + 1], scalar2=1.0,
                            op0=ALU.mult, op1=ALU.mult), reads=[sg_, gmix], writes=[Win])
            k2 = 0
            for c in (range(8) if '3' in SETUP else []):
                sg_ = stg2[k2 % 2]; k2 += 1
                load(sg_, sg_[:], wout_d[c * 128:(c + 1) * 128, :], "w2%d" % (k2 % 2))
                S.op('act', lambda a, sg_=sg_, c=c: a.copy(out=Wout[:, c, :], in_=sg_[:]), reads=[sg_], writes=[Wout])
            for c in (range(8) if '3' in SETUP else []):
                sg_ = stg2[k2 % 2]; k2 += 1
                load(sg_, sg_[:], wg_d[c * 128:(c + 1) * 128, :], "w2%d" % (k2 % 2))
                S.op('dve', lambda v, sg_=sg_, c=c: v.tensor_scalar(out=Wg[:, c, :], in0=sg_[:], scalar1=gple[:, c:c + 1],
                                                                    scalar2=None, op0=ALU.mult), reads=[sg_, gple], writes=[Wg])
            for c in (range(2) if '3' in SETUP else []):
                sg_ = stg2[k2 % 2]; k2 += 1
                load(sg_, sg_[:], wp_d[c * 128:(c + 1) * 128, :], "w2%d" % (k2 % 2))
                S.op('act', lambda a, sg_=sg_, c=c: a.activation(out=Wp[:, c, :], in_=sg_[:], func=AF.Copy, scale=0.5),
                     reads=[sg_], writes=[Wp])
            S.op('pool', lambda g: g.memset(WukT[:], 0.0), writes=[WukT])
            for a_ in (range(3) if '4' in SETUP else []):
                S.op('pe', lambda t, a_=a_: t.transpose(out=PW[0][:, a_ * 128:(a_ + 1) * 128], in_=wukr[:, a_, :],
                                                        identity=identf[:]), reads=[wukr, identf], writes=[PW[0]])
            if '4' in SETUP:
              S.op('dve', lambda v: v.tensor_scalar(out=WukT.ap(0, 128, 16, [[64, 8], [1, 48]]),
                                                  in0=PW[0].ap(0, 128, 0, [[48, 8], [1, 48]]),
                                                  scalar1=gkv[:, 0:1], scalar2=0.125, op0=ALU.mult, op1=ALU.mult),
                 reads=[PW[0], gkv, WukT], writes=[WukT])
            if '5' in SETUP:
              S.op('dve', lambda v: v.tensor_scalar(out=Wuv.ap(0, 128, 0, [[1, 512]]), in0=wuvf.ap(0, 128, 0, [[1, 512]]),
                                                  scalar1=gkv[:, 0:1], scalar2=0.5, op0=ALU.mult, op1=ALU.mult),
                 reads=[wuvf, gkv], writes=[Wuv])
            S.op('dve', lambda v: v.tensor_copy(out=posf[:], in_=posi[:]), reads=[posi], writes=[posf])
            for j in range(NT):
                S.op('dve', lambda v, j=j: v.tensor_scalar(out=U[:, j * 44:(j + 1) * 44], in0=invf[:],
                                                           scalar1=posf[:, j:j + 1], scalar2=1.0 / TWO_PI,
                                                           op0=ALU.mult, op1=ALU.mult), reads=[invf, posf], writes=[U])
            flat = lambda t: t.ap(0, 128, 0, [[1, NT * 44]])
            for kind in (("sin", "cos") if '6' in SETUP else ()):
                if kind == "cos":
                    S.op('dve', lambda v: v.tensor_scalar(out=U[:], in0=U[:], scalar1=0.25, scalar2=None, op0=ALU.add),
                         reads=[U], writes=[U])
                S.op('dve', lambda v: v.tensor_scalar(out=U1[:], in0=U[:], scalar1=MAGIC, scalar2=None, op0=ALU.add),
                     reads=[U], writes=[U1])
                S.op('dve', lambda v: v.tensor_scalar(out=U2[:], in0=U1[:], scalar1=-MAGIC, scalar2=None, op0=ALU.add),
                     reads=[U1], writes=[U2])
                S.op('dve', lambda v: v.tensor_tensor(out=U1[:], in0=U[:], in1=U2[:], op=ALU.subtract),
                     reads=[U, U2, U1], writes=[U1])
                dst = SIN if kind == "sin" else COS
                S.op('act', lambda a, dst=dst: a.activation(out=flat(dst), in_=U1[:], func=AF.Sin, scale=TWO_PI * (1 - 1e-6)),
                     reads=[U1], writes=[dst])
            S.barrier()
            S.emit(block)

        ssv = sb("ssv", [128, 8], F32)
        rsv = sb("rsv", [128, 8], F32)
        hbf = sb("hbf", [128, DM], BF16)
        hT = sb("hT", [128, 8, 128], BF16)
        rqf = sb("rqf", [128, 512], F32)
        tmpA = sb("tmpA", [128, 512], F32)
        tmpB = sb("tmpB", [128, 512], F32)
        rqb = sb("rqb", [128, 512], BF16)
        rkb = sb("rkb", [128, 512], BF16)
        qT = sb("qT", [128, 4, 128], BF16)
        qxT = sb("qxT", [128, 4, 128], BF16)
        kT = sb("kT", [128, 4, 128], BF16)
        kz = sb("kz", [128, 512], BF16)
        vb = sb("vb", [128, 512], BF16)
        thr_ = tmpA
        sgr = sb("sgr", [128, 512], BF16)
        qdb = rkb
        dqf = sb("dqf", [128, 8, 16], F32)
        dqa = tmpA
        dqb = tmpB
        sm = sb("sm", [128, 440], F32)
        smA = sb("smA", [128, 64], F32)
        smB = sb("smB", [128, 64], F32)
        kpf = sb("kpf", [128, 16], F32)
        nb = sb("nb", [128, 128], BF16)
        kpeb = sb("kpeb", [128, 16], BF16)
        ik4 = sb("ik4", [128, 4, 32], BF16)
        iqb = sb("iqb", [128, 256], BF16)
        diag = sb("diag", [128, 8, 128], BF16)
        nT = sb("nT", [128, 128], BF16)
        kpeT = sb("kpeT", [16, 128], BF16)
        qiT = sb("qiT", [128, 3, 128], BF16)
        PT = [sb("PT%d" % i, [128, 128], BF16) for i in range(4)]
        PTf = [sb("PTf%d" % i, [128, 128], BF16) for i in range(4)]
        of_ = rqf
        osq = tmpA
        gs1 = sb("gs1", [128, 8], F32)
        gs2 = sb("gs2", [128, 8], F32)
        gmean = sb("gmean", [128, 8], F32)
        gvar = sb("gvar", [128, 8], F32)
        grstd = sb("grstd", [128, 8], F32)
        retb = rqb
        Rb = [sb("Rb%d" % i, [128, 512], BF16) for i in range(2)]
        XT = [sb("xt%d" % i, [128, DM], F32) for i in range(2)]
        PTL = [sb("pt%d" % i, [128, PLE], F32) for i in range(2)]
        QTs = [sb("QT%d" % i, [128, 8, 128], BF16) for i in range(2)]
        SGD = [sb("sgd%d" % i, [128, 512], BF16) for i in range(2)]
        MIXT = [sb("mixT%d" % i, [128, 8, 128], BF16) for i in range(2)]
        SC = sb("SC", [128, SEQ], F32)
        amp = sb("amp", [128, 8], F32)
        MB = sb("MB", [128, SEQ], BF16)
        DL = sb("DL", [128, n_bis + 1], F32)
        mids = sb("mids", [128, n_bis + 2], F32)
        cnts = sb("cnts", [128, n_bis + 1], F32)
        sgs = sb("sgs", [128, n_bis + 1], F32)
        Eb = [sb("Eb%d" % i, [128, 512], BF16) for i in range(2)]
        rec = sb("rec", [128, 4], F32)
        dsab = sb("dsab", [128, 512], BF16)
        ssvB = sb("ssvB", [128, 8], F32)
        rsvB = sb("rsvB", [128, 8], F32)
        hbf2 = sb("hbf2", [128, DM], BF16)
        h2T = sb("h2T", [128, 8, 128], BF16)
        th2 = sb("th2", [128, 512], F32)
        pbf = sb("pbf", [128, PLE], BF16)
        pT = sb("pT", [128, 2, 128], BF16)

        def rms_rstd(src_ap, n, col, reads, junk, ssv_, rsv_):
            S.op('act', lambda a: a.activation(out=junk[:, 0:n], in_=src_ap, func=AF.Square, accum_out=ssv_[:, col:col + 1]),
                 reads=reads, writes=[junk, ssv_])
            S.op('pool', lambda g: g.tensor_scalar(out=ssv_[:, col:col + 1], in0=ssv_[:, col:col + 1], scalar1=1.0 / n,
                                                   scalar2=EPS, op0=ALU.mult, op1=ALU.add), reads=[ssv_], writes=[ssv_])
            S.op('pool', lambda g: g.tensor_tensor(out=rsv_[:, col:col + 1], in0=ssv_[:, col:col + 1], in1=mhalf[:, 0:1],
                                                   op=ALU.pow), reads=[ssv_, mhalf], writes=[rsv_])

        def transposes(src_list, dst_ap_fn, reads, writes, base=0):
            pb = P2a if base == 0 else P2b
            for k, a_in in enumerate(src_list):
                S.op('pe', lambda t, k=k, a_in=a_in: t.transpose(out=P2[:, base + k, :], in_=a_in, identity=identb[:]),
                     reads=reads + [identb], writes=[pb] if len(src_list) <= 4 else [P2a, P2b])
            n = len(src_list)
            S.op('act', lambda a: a.copy(out=dst_ap_fn(), in_=P2[:, base:base + n, :]),
                 reads=[pb] if n <= 4 else [P2a, P2b], writes=writes)

        def V4(t, off, hs, nh, half):
            return bass.AP(t.t, off, [[t.row, 128], [hs, nh], [half, 2], [1, half]])

        def V3(t, off, hs, nh, half):
            return bass.AP(t.t, off, [[t.row, 128], [hs, nh], [1, half]])

        def rotary(eng, src, soff, shs, dst, doff, dhs, ta, tb, nh, half, tcol, j):
            def tb4(t):
                return bass.AP(t.t, j * 44 + tcol, [[NT * 44, 128], [0, nh], [0, 2], [1, half]])

            def tb3(t):
                return bass.AP(t.t, j * 44 + tcol, [[NT * 44, 128], [0, nh], [1, half]])
            ths = 2 * half
            S.op(eng, lambda g: g.tensor_tensor(out=V4(ta, 0, ths, nh, half), in0=V4(src, soff, shs, nh, half),
                                                in1=tb4(COS), op=ALU.mult), reads=[src, COS], writes=[ta])
            S.op(eng, lambda g: g.tensor_tensor(out=V3(tb, 0, ths, nh, half), in0=V3(src, soff + half, shs, nh, half),
                                                in1=tb3(SIN), op=ALU.mult), reads=[src, SIN], writes=[tb])
            S.op(eng, lambda g: g.tensor_tensor(out=V3(tb, half, ths, nh, half), in0=V3(src, soff, shs, nh, half),
                                                in1=tb3(SIN), op=ALU.mult), reads=[src, SIN, tb], writes=[tb])
            S.op(eng, lambda g: g.tensor_tensor(out=V3(dst, doff, dhs, nh, half), in0=V3(ta, 0, ths, nh, half),
                                                in1=V3(tb, 0, ths, nh, half), op=ALU.subtract),
                 reads=[ta, tb, dst], writes=[dst])
            S.op(eng, lambda g: g.tensor_tensor(out=V3(dst, doff + half, dhs, nh, half), in0=V3(ta, half, ths, nh, half),
                                                in1=V3(tb, half, ths, nh, half), op=ALU.add),
                 reads=[ta, tb, dst], writes=[dst])

        unit = [0]
        out_toks = []
        tdone = [-1]
        for i_ in range(2):
            S.op('pool', lambda g, i_=i_: g.memset(QTs[i_][:], 0.0), writes=[QTs[i_]])

        def stage_A(q, j, k):
            xt = XT[k % 2]; pt = PTL[k % 2]; QT = QTs[k % 2]; sgd = SGD[k % 2]; mixT = MIXT[k % 2]
            r0 = j * 128
            n = 128 * (j + 1)
            if j == 0:
                S.op('pool', lambda g: g.memset(st[:], 0.0), writes=[st])
                S.op('pool', lambda g: g.memset(stb[:], 0.0), writes=[stb])
            S.dma('sp', lambda qq: qq.dma_start(out=xt[:], in_=x_d[q, r0:r0 + 128, :]), "ldx%d" % (k % 2), writes=[xt])
            S.dma('sp', lambda qq: qq.dma_start(out=pt[:], in_=p_d[q, r0:r0 + 128, :]), "ldp%d" % (k % 2), writes=[pt])
            rms_rstd(xt[:], DM, 0, [xt], hbf, ssv, rsv)
            S.op('act', lambda a: a.activation(out=hbf[:], in_=xt[:], func=AF.Identity, scale=rsv[:, 0:1]),
                 reads=[xt, rsv], writes=[hbf])
            transposes([hbf[:, c * 128:(c + 1) * 128] for c in range(8)], lambda: hT[:], [hbf], [hT])
            yield

            def proj(g, width):
                bank = wide()
                for c in range(8):
                    S.op('pe', lambda t, c=c, bank=bank: t.matmul(bank[:, 0:width], lhsT=hT[:, c, :],
                                                                  rhs=Win[:, c, g * 512:g * 512 + width],
                                                                  start=(c == 0), stop=(c == 7)),
                         reads=[hT, Win], writes=[bank])
                return bank

            bank = proj(6, 440)
            S.op('act', lambda a, bank=bank: a.copy(out=sm[:], in_=bank[:, 0:440]), reads=[bank], writes=[sm])
            rms_rstd(sm[:, 0:128], 128, 1, [sm], hbf, ssv, rsv)
            S.op('pool', lambda g: g.tensor_scalar(out=nb[:], in0=sm[:, 0:128], scalar1=rsv[:, 1:2], scalar2=1.0,
                                                   op0=ALU.mult, op1=ALU.mult), reads=[sm, rsv], writes=[nb])
            rotary('pool', sm, 128, 16, kpf, 0, 16, smA, smB, 1, 8, 32, j)
            S.op('pool', lambda g: g.tensor_scalar(out=kpeb[:], in0=kpf[:], scalar1=0.125, scalar2=1.0,
                                                   op0=ALU.mult, op1=ALU.mult), reads=[kpf], writes=[kpeb])
            rotary('pool', sm, 400, 8, sm, 400, 8, smA, smB, 1, 4, 40, j)
            S.op('pool', lambda g: g.tensor_copy(out=ik4[:], in_=sm.ap(0, 128, 400, [[0, 4], [1, 32]])),
                 reads=[sm], writes=[ik4])
            rotary('pool', sm, 144, 32, sm, 144, 32, smA, smB, 8, 4, 40, j)
            S.op('pool', lambda g: g.tensor_copy(out=iqb[:], in_=sm[:, 144:400]), reads=[sm], writes=[iqb])
            S.op('pool', lambda g: g.tensor_tensor(out=diag[:], in0=identw.ap(0, 128, 0, [[0, 8], [1, 128]]),
                                                   in1=sm.ap(0, 128, 432, [[1, 8], [0, 128]]), op=ALU.mult),
                 reads=[identw, sm], writes=[diag])
            yield
            S.op('pe', lambda t: t.transpose(out=P2[:, 0, :], in_=nb[:], identity=identb[:]), reads=[nb, identb], writes=[P2a])
            S.op('pe', lambda t: t.transpose(out=P2[0:16, 1, :], in_=kpeb[:], identity=identb[:]),
                 reads=[kpeb, identb], writes=[P2a])
            S.op('pe', lambda t: t.transpose(out=P2[:, 2, :], in_=ik4.ap(0, 128, 0, [[1, 128]]), identity=identb[:]),
                 reads=[ik4, identb], writes=[P2a])
            S.op('pe', lambda t: t.transpose(out=P2[0:96, 4, :], in_=iqb[:, 0:96], identity=identb[:]),
                 reads=[iqb, identb], writes=[P2b])
            S.op('pe', lambda t: t.transpose(out=P2[0:96, 5, :], in_=iqb[:, 96:192], identity=identb[:]),
                 reads=[iqb, identb], writes=[P2b])
            S.op('pe', lambda t: t.transpose(out=P2[0:64, 6, :], in_=iqb[:, 192:256], identity=identb[:]),
                 reads=[iqb, identb], writes=[P2b])
            S.op('act', lambda a: a.copy(out=nT[:], in_=P2[:, 0, :]), reads=[P2a], writes=[nT])
            S.op('act', lambda a: a.copy(out=kpeT[:], in_=P2[0:16, 1, :]), reads=[P2a], writes=[kpeT])
            S.op('act', lambda a: a.copy(out=KIall[:, j * 128:(j + 1) * 128], in_=P2[:, 2, :]), reads=[P2a], writes=[KI[j]])
            S.op('act', lambda a: a.copy(out=qiT[0:96, 0:2, :], in_=P2[0:96, 4:6, :]), reads=[P2b], writes=[qiT])
            S.op('act', lambda a: a.copy(out=qiT[0:64, 2, :], in_=P2[0:64, 6, :]), reads=[P2b], writes=[qiT])
            yield
            for p in range(4):
                S.op('pe', lambda t, p=p: t.matmul(P3[:, p, :], lhsT=WukT.ap(0, 128, p * 128, [[1, 128]]), rhs=nT[:],
                                                   start=True, stop=False), reads=[WukT, nT], writes=[P3])
                S.op('pe', lambda t, p=p: t.matmul(P3[:, p, :], lhsT=ipe[:], rhs=kpeT[:], start=False, stop=True),
                     reads=[ipe, kpeT], writes=[P3])
            S.op('act', lambda a: a.copy(out=KT[j][:], in_=P3[:]), reads=[P3], writes=[KT[j]])
            bank = wide()
            S.op('pe', lambda t, bank=bank: t.matmul(bank[:], lhsT=nT[:], rhs=Wuv.ap(0, 128, 0, [[1, 512]]),
                                                     start=True, stop=True), reads=[nT, Wuv], writes=[bank])
            S.op('act', lambda a, bank=bank: a.copy(out=VX[j].ap(0, 128, 0, [[66, 8], [1, 64]]),
                                                    in_=bank.ap(0, 128, 0, [[64, 8], [1, 64]])),
                 reads=[bank], writes=[VX[j]])
            yield
            for which in (0, 1):
                bank = proj(which, 512)
                S.op('act', lambda a, bank=bank: a.copy(out=rqf[:], in_=bank[:]), reads=[bank], writes=[rqf])
                yield
                dstb = rqb if which == 0 else rkb
                rotary('pool', rqf, 0, 64, dstb, 0, 64, tmpA, tmpB, 8, 32, 0, j)
                yield
                if which == 0:
                    transposes([rqb[:, c * 128:(c + 1) * 128] for c in range(4)], lambda: qT[:], [rqb], [qT])
                    S.op('pool', lambda g: g.tensor_tensor(out=qxT[:], in0=qT[:], in1=xiT[:], op=ALU.mult),
                         reads=[qT, xiT], writes=[qxT])
                else:
                    transposes([rkb[:, c * 128:(c + 1) * 128] for c in range(4)], lambda: kT[:], [rkb], [kT], base=4)
                    S.op('pool', lambda g: g.tensor_tensor(
                        out=kz.ap(0, 128, 0, [[64, 8], [1, 64]]), in0=rkb.ap(0, 128, 0, [[64, 8], [1, 64]]),
                        in1=zeta.ap(0, 128, 0, [[1, 8], [0, 64]]), op=ALU.mult), reads=[rkb, zeta], writes=[kz])
                yield
            bank = proj(2, 512)
            S.op('act', lambda a, bank=bank: a.copy(out=vb[:], in_=bank[:]), reads=[bank], writes=[vb])
            yield
            for (g, dst) in ((3, sgr), (5, sgd)):
                bank = proj(g, 512)
                S.op('act', lambda a, bank=bank: a.activation(out=thr_[:], in_=bank[:], func=AF.Tanh, scale=0.5),
                     reads=[bank], writes=[thr_])
                if '1' in XF:
                    S.op('act', lambda a, bank=bank: a.copy(out=rqf[:], in_=bank[:]), reads=[bank], writes=[rqf])
                    S.op('pool', lambda g_: g_.tensor_tensor(out=tmpB[:], in0=thr_[:], in1=rqf[:], op=ALU.mult),
                         reads=[thr_, rqf], writes=[tmpB])
                    S.op('pool', lambda g_, dst=dst: g_.tensor_tensor(out=dst[:], in0=tmpB[:], in1=rqf[:], op=ALU.add),
                         reads=[tmpB, rqf], writes=[dst])
                else:
                    S.op('dve', lambda v, bank=bank, dst=dst: v.scalar_tensor_tensor(
                        out=dst[:], in0=thr_[:], scalar=1.0, in1=bank[:], op0=ALU.add, op1=ALU.mult),
                        reads=[thr_, bank], writes=[dst])
                yield
            bank = proj(4, 512)
            S.op('act', lambda a, bank=bank: a.copy(out=qdb[:], in_=bank[:]), reads=[bank], writes=[qdb])
            S.op('act', lambda a, bank=bank: a.copy(out=dqf[:], in_=bank.ap(0, 128, 0, [[64, 8], [1, 16]])),
                 reads=[bank], writes=[dqf])
            rotary('pool', dqf, 0, 16, qdb, 0, 64, dqa, dqb, 8, 8, 32, j)
            for c_ in range(4):
                S.op('pe', lambda t, c_=c_: t.transpose(out=P2[:, c_, :], in_=qdb[:, c_ * 128:(c_ + 1) * 128], identity=identb[:]),
                     reads=[qdb, identb], writes=[P2a])
            S.op('act', lambda a: a.copy(out=QT.ap(0, 64, 0, [[256, 4], [1, 128]]), in_=P2[0:64, 0:4, :]), reads=[P2a], writes=[QT])
            S.op('act', lambda a: a.copy(out=QT.ap(64, 64, 128, [[256, 4], [1, 128]]), in_=P2[64:128, 0:4, :]), reads=[P2a], writes=[QT])
            yield
            obank = wide()
            for h in range(8):
                p, e = divmod(h, 2)
                rr = slice(e * 64, (e + 1) * 64)
                r4 = h % 4
                S.op('pe', lambda t, p=p, rr=rr, r4=r4: t.matmul(P4[:, r4, :], lhsT=kT[rr, p, :], rhs=qT[rr, p, :],
                                                                 start=True, stop=True), reads=[kT, qT], writes=[P4])
                if '2' in XF:
                    S.op('act', lambda a, r4=r4: a.copy(out=PTf[r4][:], in_=P4[:, r4, :]), reads=[P4], writes=[PTf[r4]])
                    S.op('pool', lambda g_, h=h, r4=r4: g_.tensor_tensor(out=PT[r4][:], in0=PTf[r4][:], in1=maskT[:, h, :],
                                                                         op=ALU.mult), reads=[PTf[r4], maskT], writes=[PT[r4]])
                else:
                    S.op('dve', lambda v, h=h, r4=r4: v.tensor_tensor(out=PT[r4][:], in0=P4[:, r4, :], in1=maskT[:, h, :],
                                                                      op=ALU.mult), reads=[P4, maskT], writes=[PT[r4]])
                S.op('pe', lambda t, h=h, r4=r4, obank=obank: t.matmul(obank[:, h * 64:(h + 1) * 64], lhsT=PT[r4][:],
                                                                      rhs=vb[:, h * 64:(h + 1) * 64], start=True, stop=False),
                     reads=[PT[r4], vb], writes=[obank])
                S.op('pe', lambda t, h=h, p=p, rr=rr, obank=obank: t.matmul(obank[:, h * 64:(h + 1) * 64], lhsT=qxT[rr, p, :],
                                                                           rhs=stb[rr, p, :], start=False, stop=True),
                     reads=[qxT, stb], writes=[obank])
            for p in range(4):
                S.op('pe', lambda t, p=p: t.matmul(P3[:, p, :], lhsT=kz[:, p * 128:(p + 1) * 128], rhs=vb[:, p * 128:(p + 1) * 128],
                                                   start=True, stop=True), reads=[kz, vb], writes=[P3])
            S.op('pool', lambda g: g.tensor_tensor(out=st[:], in0=st[:], in1=decay[:], op=ALU.mult),
                 reads=[st, decay], writes=[st])
            if '3' in XF:
                S.op('act', lambda a: a.copy(out=tmpB.ap(0, 64, 0, [[64, 4], [1, 64]]), in_=P3[0:64, :, 0:64]), reads=[P3], writes=[tmpB])
                S.op('act', lambda a: a.copy(out=tmpB.ap(64, 64, 0, [[64, 4], [1, 64]]), in_=P3[64:128, :, 64:128]), reads=[P3], writes=[tmpB])
                S.op('pool', lambda g: g.tensor_tensor(out=st.ap(0, 128, 0, [[1, 256]]), in0=st.ap(0, 128, 0, [[1, 256]]),
                                                       in1=tmpB[:, 0:256], op=ALU.add), reads=[st, tmpB], writes=[st])
            else:
                S.op('dve', lambda v: v.tensor_tensor(out=st[0:64, :, :], in0=st[0:64, :, :], in1=P3[0:64, :, 0:64], op=ALU.add),
                     reads=[st, P3], writes=[st])
                S.op('dve', lambda v: v.tensor_tensor(out=st[64:128, :, :], in0=st[64:128, :, :], in1=P3[64:128, :, 64:128],
                                                      op=ALU.add), reads=[st, P3], writes=[st])
            S.op('pool', lambda g: g.tensor_copy(out=stb[:], in_=st[:]), reads=[st], writes=[stb])
            if '4' in XF:
                for h in range(8):
                    S.op('act', lambda a, obank=obank, h=h: a.activation(out=of_[:, h * 64:(h + 1) * 64], in_=obank[:, h * 64:(h + 1) * 64],
                                                                         func=AF.Identity, accum_out=gs1[:, h:h + 1]),
                         reads=[obank], writes=[of_, gs1])
                    S.op('act', lambda a, obank=obank, h=h: a.activation(out=osq[:, h * 64:(h + 1) * 64], in_=obank[:, h * 64:(h + 1) * 64],
                                                                         func=AF.Square, accum_out=gs2[:, h:h + 1]),
                         reads=[obank], writes=[osq, gs2])
                o3 = lambda t: t.ap(0, 128, 0, [[64, 8], [1, 64]])
                bc8 = lambda t: t.ap(0, 128, 0, [[1, 8], [0, 64]])
                S.op('pool', lambda g: g.tensor_scalar(out=gmean[:], in0=gs1[:], scalar1=1.0 / 64, scalar2=1.0, op0=ALU.mult, op1=ALU.mult),
                     reads=[gs1], writes=[gmean])
                S.op('pool', lambda g: g.tensor_tensor(out=gs1[:], in0=gmean[:], in1=gmean[:], op=ALU.mult), reads=[gmean, gs1], writes=[gs1])
                S.op('pool', lambda g: g.tensor_scalar(out=gs2[:], in0=gs2[:], scalar1=1.0 / 64, scalar2=EPS, op0=ALU.mult, op1=ALU.add),
                     reads=[gs2], writes=[gs2])
                S.op('pool', lambda g: g.tensor_tensor(out=gvar[:], in0=gs2[:], in1=gs1[:], op=ALU.subtract), reads=[gs1, gs2], writes=[gvar])
            else:
                S.op('act', lambda a, obank=obank: a.copy(out=of_[:], in_=obank[:]), reads=[obank], writes=[of_])
                S.op('act', lambda a, obank=obank: a.activation(out=osq[:], in_=obank[:], func=AF.Square), reads=[obank], writes=[osq])
                o3 = lambda t: t.ap(0, 128, 0, [[64, 8], [1, 64]])
                bc8 = lambda t: t.ap(0, 128, 0, [[1, 8], [0, 64]])
                S.op('dve', lambda v: v.tensor_reduce(out=gs1[:], in_=o3(of_), axis=AX.X, op=ALU.add), reads=[of_], writes=[gs1])
                S.op('dve', lambda v: v.tensor_reduce(out=gs2[:], in_=o3(osq), axis=AX.X, op=ALU.add), reads=[osq], writes=[gs2])
                S.op('dve', lambda v: v.tensor_scalar(out=gmean[:], in0=gs1[:], scalar1=1.0 / 64, scalar2=None, op0=ALU.mult),
                     reads=[gs1], writes=[gmean])
                S.op('dve', lambda v: v.tensor_tensor(out=gs1[:], in0=gmean[:], in1=gmean[:], op=ALU.mult), reads=[gmean, gs1], writes=[gs1])
                S.op('dve', lambda v: v.tensor_scalar(out=gs2[:], in0=gs2[:], scalar1=1.0 / 64, scalar2=EPS, op0=ALU.mult, op1=ALU.add),
                     reads=[gs2], writes=[gs2])
                S.op('dve', lambda v: v.tensor_tensor(out=gvar[:], in0=gs2[:], in1=gs1[:], op=ALU.subtract), reads=[gs1, gs2], writes=[gvar])
                S.op('dve', lambda v: v.tensor_scalar(out=gvar[:], in0=gvar[:], scalar1=1e-12, scalar2=None, op0=ALU.max),
                     reads=[gvar], writes=[gvar])
            S.op('pool', lambda g: g.tensor_tensor(out=grstd[:], in0=gvar[:], in1=mhalf[:], op=ALU.pow),
                 reads=[gvar, mhalf], writes=[grstd])
            yield
            S.op('pool', lambda g: g.tensor_tensor(out=o3(of_), in0=o3(of_), in1=bc8(gmean), op=ALU.subtract),
                 reads=[of_, gmean], writes=[of_])
            S.op('pool', lambda g: g.tensor_tensor(out=o3(of_), in0=o3(of_), in1=bc8(grstd), op=ALU.mult),
                 reads=[of_, grstd], writes=[of_])
            S.op('pool', lambda g: g.tensor_tensor(out=osq[:], in0=gret[:], in1=sgr[:], op=ALU.mult),
                 reads=[gret, sgr, osq], writes=[osq])
            S.op('pool', lambda g: g.tensor_tensor(out=retb[:], in0=of_[:], in1=osq[:], op=ALU.mult),
                 reads=[of_, osq], writes=[retb])
            transposes([retb[:, c * 128:(c + 1) * 128] for c in range(4)], lambda: mixT[:, 0:4, :], [retb], [mixT], base=4)
            yield
            while tdone[0] < k - 1:
                yield
            if j >= 2:
                ng = (n + 511) // 512
                for sgi in range(ng):
                    w = min(512, n - 512 * sgi)
                    c0 = sgi * 512
                    kbufs = [KI[t_] for t_ in range(c0 // 128, (c0 + w) // 128)]
                    def emitZ(h, c0=c0, w=w, kbufs=kbufs):
                        c4, r4 = divmod(h, 3)
                        rr = slice(r4 * 32, (r4 + 1) * 32)
                        zb = PW[h % 2]
                        S.op('pe', lambda t, zb=zb, rr=rr, c4=c4: t.matmul(
                            zb[:, 0:w], lhsT=qiT[rr, c4, :], rhs=KIall[rr, c0:c0 + w], start=True, stop=True),
                            reads=[qiT] + kbufs, writes=[zb])
                    emitZ(0)
                    for h in range(8):
                        if h + 1 < 8:
                            emitZ(h + 1)
                        zb = PW[h % 2]
                        rb = Rb[h % 2]
                        S.op('act', lambda a, zb=zb, rb=rb, w=w: a.activation(out=rb[:, 0:w], in_=zb[:, 0:w], func=AF.Relu),
                             reads=[zb], writes=[rb])
                        S.op('pe', lambda t, h=h, rb=rb, w=w: t.matmul(P4.ap(0, 128, 0, [[1, w]]), lhsT=diag[:, h, :], rhs=rb[:, 0:w],
                                                                       start=(h == 0), stop=(h == 7)),
                             reads=[diag, rb], writes=[P4])
                    S.op('act', lambda a, c0=c0, w=w: a.copy(out=SC[:, c0:c0 + w], in_=P4.ap(0, 128, 0, [[1, w]])), reads=[P4], writes=[SC])
                    yield
                    if '5' not in XF:
                        S.op('dve', lambda v, sgi=sgi, w=w, c0=c0: v.tensor_reduce(out=amp[:, sgi:sgi + 1], in_=SC[:, c0:c0 + w], axis=AX.X,
                                                                                   op=ALU.max, apply_absolute_value=True),
                             reads=[SC], writes=[amp])
                if '5' not in XF:
                    S.op('pool', lambda g: g.tensor_tensor(out=SC[:, n - 128:n], in0=SC[:, n - 128:n], in1=cb[:], op=ALU.add),
                         reads=[SC, cb], writes=[SC])
                yield

        def stage_T(q, j, k):
            xt = XT[k % 2]; pt = PTL[k % 2]; QT = QTs[k % 2]; sgd = SGD[k % 2]; mixT = MIXT[k % 2]
            r0 = j * 128
            n = 128 * (j + 1)
            if j >= 2:
                ng = (n + 511) // 512
                if '5' in XF:
                    S.op('dve', lambda v: v.tensor_reduce(out=amp[:, 7:8], in_=SC[:, 0:n], axis=AX.X, op=ALU.max, apply_absolute_value=True),
                         reads=[SC], writes=[amp])
                    S.op('dve', lambda v: v.tensor_tensor(out=SC[:, n - 128:n], in0=SC[:, n - 128:n], in1=cb[:], op=ALU.add),
                         reads=[SC, cb], writes=[SC])
                else:
                    S.op('dve', lambda v: v.tensor_reduce(out=amp[:, 7:8], in_=amp[:, 0:ng], axis=AX.X, op=ALU.max),
                         reads=[amp], writes=[amp])
                S.op('dve', lambda v: v.tensor_scalar(out=DL[:], in0=pow2[:], scalar1=amp[:, 7:8], scalar2=None, op0=ALU.mult),
                     reads=[pow2, amp], writes=[DL])
                S.op('dve', lambda v: v.tensor_scalar(out=mids[:, 0:1], in0=pow2[:, 0:1], scalar1=0.0, scalar2=None, op0=ALU.mult),
                     reads=[pow2], writes=[mids])
                for i in range(n_bis):
                    S.op('dve', lambda v, i=i: v.tensor_scalar(out=MB[:, 0:n], in0=SC[:, 0:n], scalar1=mids[:, i:i + 1],
                                                               scalar2=None, op0=ALU.is_gt, op1=ALU.add,
                                                               accum_out=cnts[:, i:i + 1]),
                         reads=[SC, mids], writes=[MB, cnts])
                    S.op('dve', lambda v, i=i: v.tensor_scalar(out=sgs[:, i:i + 1], in0=cnts[:, i:i + 1], scalar1=256.0,
                                                               scalar2=0.5, op0=ALU.is_ge, op1=ALU.subtract),
                         reads=[cnts], writes=[sgs])
                    S.op('dve', lambda v, i=i: v.scalar_tensor_tensor(out=mids[:, i + 1:i + 2], in0=sgs[:, i:i + 1],
                                                                      scalar=DL[:, i:i + 1], in1=mids[:, i:i + 1],
                                                                      op0=ALU.mult, op1=ALU.add),
                         reads=[sgs, DL, mids], writes=[mids])
                    yield
                S.op('dve', lambda v: v.tensor_tensor(out=mids[:, n_bis + 1:n_bis + 2], in0=mids[:, n_bis:n_bis + 1],
                                                      in1=DL[:, n_bis:n_bis + 1], op=ALU.subtract),
                     reads=[mids, DL], writes=[mids])
                S.op('dve', lambda v: v.tensor_scalar(out=MB[:, 0:n], in0=SC[:, 0:n], scalar1=mids[:, n_bis + 1:n_bis + 2],
                                                      scalar2=NEG, op0=ALU.is_le, op1=ALU.mult),
                     reads=[SC, mids], writes=[MB])
            else:
                if j == 1:
                    S.op('pool', lambda g: g.memset(MB[:, 0:128], 0.0), writes=[MB])
                S.op('pool', lambda g: g.tensor_copy(out=MB[:, n - 128:n], in_=cbm[:]), reads=[cbm], writes=[MB])
            tdone[0] = k
            yield

        def stage_B(q, j, k):
            xt = XT[k % 2]; pt = PTL[k % 2]; QT = QTs[k % 2]; sgd = SGD[k % 2]; mixT = MIXT[k % 2]
            r0 = j * 128
            n = 128 * (j + 1)
            for hg in range(2):
                S.op('pe', lambda t: t.matmul(P7[:, 0:264], lhsT=zt[0:32, 0:128], rhs=zt[0:32, 0:264], start=True, stop=False),
                     reads=[zt], writes=[P7])
                def emit_qk(i, u, hg=hg):
                    qk = PQ[u % 2]
                    S.op('pe', lambda t, qk=qk, i=i: t.matmul(qk[:], lhsT=MB[:, i * 128:(i + 1) * 128],
                                                              rhs=I4.ap(0, 128, 0, [[1, 512]]), start=True, stop=False),
                         reads=[MB, I4], writes=[qk])
                    for hh in range(4):
                        h = hg * 4 + hh
                        p = h // 2
                        S.op('pe', lambda t, qk=qk, hh=hh, p=p, h=h, i=i: t.matmul(
                            qk[:, hh * 128:(hh + 1) * 128], lhsT=KT[i][:, p, :], rhs=QT[:, h, :], start=False, stop=True),
                            reads=[KT[i], QT], writes=[qk])
                ubase = unit[0] + 1
                unit[0] += j + 1
                emit_qk(0, ubase)
                for i in range(j + 1):
                    u = ubase + i
                    if i + 1 <= j:
                        emit_qk(i + 1, u + 1)
                    qk = PQ[u % 2]
                    eb = Eb[u % 2]
                    S.op('act', lambda a, qk=qk, eb=eb: a.activation(out=eb[:], in_=qk[:], func=AF.Exp), reads=[qk], writes=[eb])
                    for hh in range(4):
                        h = hg * 4 + hh
                        S.op('pe', lambda t, eb=eb, hh=hh, h=h, i=i: t.matmul(
                            P7[:, hh * 66:hh * 66 + 66], lhsT=eb[:, hh * 128:(hh + 1) * 128], rhs=VX[i][:, h, :],
                            start=False, stop=(i == j)), reads=[eb, VX[i]], writes=[P7])
                    yield
                S.op('dve', lambda v: v.reciprocal(out=rec[:], in_=P7.ap(0, 128, 64, [[66, 4]])), reads=[P7], writes=[rec])
                for hh in range(4):
                    h = hg * 4 + hh
                    S.op('dve', lambda v, hh=hh, h=h: v.scalar_tensor_tensor(
                        out=dsab[:, h * 64:(h + 1) * 64], in0=P7[:, hh * 66:hh * 66 + 64], scalar=rec[:, hh:hh + 1],
                        in1=sgd[:, h * 64:(h + 1) * 64], op0=ALU.mult, op1=ALU.mult), reads=[P7, rec, sgd, dsab], writes=[dsab])
                yield
            transposes([dsab[:, c * 128:(c + 1) * 128] for c in range(4)], lambda: mixT[:, 4:8, :], [dsab], [mixT])
            yield
            for g in range(2):
                bank = wide()
                for c in range(8):
                    S.op('pe', lambda t, c=c, g=g, bank=bank: t.matmul(bank[:], lhsT=mixT[:, c, :],
                                                                      rhs=Wout[:, c, g * 512:(g + 1) * 512],
                                                                      start=(c == 0), stop=(c == 7)),
                         reads=[mixT, Wout], writes=[bank])
                S.op('dve', lambda v, g=g, bank=bank: v.tensor_tensor(out=xt[:, g * 512:(g + 1) * 512], in0=bank[:],
                                                                     in1=xt[:, g * 512:(g + 1) * 512], op=ALU.add),
                     reads=[bank, xt], writes=[xt])
                yield
            rms_rstd(xt[:], DM, 2, [xt], hbf2, ssvB, rsvB)
            S.op('act', lambda a: a.activation(out=hbf2[:], in_=xt[:], func=AF.Identity, scale=rsvB[:, 2:3]),
                 reads=[xt, rsvB], writes=[hbf2])
            transposes([hbf2[:, c * 128:(c + 1) * 128] for c in range(8)], lambda: h2T[:], [hbf2], [h2T])
            yield
            S.op('act', lambda a: a.copy(out=pbf[:], in_=pt[:]), reads=[pt], writes=[pbf])
            S.op('pe', lambda t: t.transpose(out=P2[:, 0, :], in_=pbf[:, 0:128], identity=identb[:]), reads=[pbf, identb], writes=[P2a])
            S.op('pe', lambda t: t.transpose(out=P2[:, 1, :], in_=pbf[:, 128:256], identity=identb[:]), reads=[pbf, identb], writes=[P2a])
            S.op('act', lambda a: a.copy(out=pT[:], in_=P2[:, 0:2, :]), reads=[P2a], writes=[pT])
            for g in range(2):
                gb = wide()
                for c in range(8):
                    S.op('pe', lambda t, c=c, g=g, gb=gb: t.matmul(gb[:], lhsT=h2T[:, c, :], rhs=Wg[:, c, g * 512:(g + 1) * 512],
                                                                  start=(c == 0), stop=(c == 7)), reads=[h2T, Wg], writes=[gb])
                S.op('act', lambda a, gb=gb: a.activation(out=th2[:], in_=gb[:], func=AF.Tanh, scale=0.5), reads=[gb], writes=[th2])
                pb = wide()
                for c in range(2):
                    S.op('pe', lambda t, c=c, g=g, pb=pb: t.matmul(pb[:], lhsT=pT[:, c, :], rhs=Wp[:, c, g * 512:(g + 1) * 512],
                                                                  start=(c == 0), stop=(c == 1)), reads=[pT, Wp], writes=[pb])
                S.op('dve', lambda v, pb=pb: v.scalar_tensor_tensor(out=th2[:], in0=th2[:], scalar=1.0, in1=pb[:],
                                                                    op0=ALU.add, op1=ALU.mult), reads=[th2, pb], writes=[th2])
                S.op('pool', lambda gg, g=g: gg.tensor_tensor(out=xt[:, g * 512:(g + 1) * 512], in0=xt[:, g * 512:(g + 1) * 512],
                                                              in1=th2[:], op=ALU.add), reads=[xt, th2], writes=[xt])
                yield
            rms_rstd(xt[:], DM, 3, [xt], hbf2, ssvB, rsvB)
            S.op('dve', lambda v: v.scalar_tensor_tensor(out=xt[:], in0=xt[:], scalar=rsvB[:, 3:4], in1=gfin[:],
                                                         op0=ALU.mult, op1=ALU.mult), reads=[xt, rsvB, gfin], writes=[xt])
            tok = S.dma('sp', lambda qq: qq.dma_start(out=out_d[q, r0:r0 + 128, :], in_=xt[:]), "sto%d" % (k % 2), reads=[xt])
            out_toks.append(tok)
            yield

        def run(gens):
            gens = list(gens)
            while gens:
                for g_ in list(gens):
                    try:
                        next(g_)
                    except StopIteration:
                        gens.remove(g_)

        def advance(g_, nsteps):
            for _ in range(nsteps):
                try:
                    next(g_)
                except StopIteration:
                    return False
            return True

        kk = 0
        for q in range(nseq):
            run([stage_A(q, 0, kk)])
            for j in range(NTILES):
                gb = stage_B(q, j, kk)
                if A_HEAD >= 0:
                    run([stage_T(q, j, kk)])
                    if j + 1 < NTILES:
                        ga = stage_A(q, j + 1, kk + 1)
                        if advance(ga, A_HEAD):
                            run([ga, gb])
                        else:
                            run([gb])
                    else:
                        run([gb])
                else:
                    gt = stage_T(q, j, kk)
                    def tb():
                        yield from gt
                        yield from gb
                    if j + 1 < NTILES:
                        run([stage_A(q, j + 1, kk + 1), tb()])
                    else:
                        run([tb()])
                kk += 1
        for tok in out_toks[-2:]:
            S.wait_tok('sp', tok)
        S.emit(block)
    return nc


def kernel(x, p, positions, norm_mix, w_in, ret_norm, kv_norm, w_uk, w_uv, w_out,
           norm_ple, w_ple_gate, w_ple_proj, norm_final, _nseq=None, _ncores=N_CORES):
    f = lambda a: np.ascontiguousarray(np.asarray(a, dtype=np.float32))
    x = f(x)
    p = f(p)[0]
    B = x.shape[0]
    nseq = B // _ncores
    consts = host_consts()
    shared = {
        "positions": np.ascontiguousarray(np.asarray(positions, dtype=np.int32)),
        "norm_mix": f(norm_mix)[0], "w_in": f(w_in)[0], "ret_norm": f(ret_norm)[0], "kv_norm": f(kv_norm)[0],
        "w_uk": f(w_uk)[0].reshape(384, 128), "w_uv": f(w_uv)[0], "w_out": f(w_out)[0], "norm_ple": f(norm_ple)[0],
        "w_ple_gate": f(w_ple_gate)[0], "w_ple_proj": f(w_ple_proj)[0], "norm_final": f(norm_final),
    }
    shared.update(consts)
    nc = build_nc(nseq)
    in_maps = []
    for c in range(_ncores):
        d = dict(shared)
        d["x"] = np.ascontiguousarray(x[c * nseq:(c + 1) * nseq])
        d["p"] = np.ascontiguousarray(p[c * nseq:(c + 1) * nseq])
        in_maps.append(d)
    res = run_bass_kernel_spmd(nc, in_maps, core_ids=list(range(_ncores)))
    out = np.concatenate([np.asarray(r["out"], dtype=np.float32) for r in res.results], axis=0)
    return out
```
